# Optimizing a Trainium2 kernel written in Bass

```python
import functools
import jax, jax.numpy as jnp
from jax import lax
import numpy as np

D_MODEL = 1024
BATCH = 2
SEQ = 8192
DEPTH = 1
DEC_BATCH = 128
DEC_SEQ = 4
PAST_LEN = 2048
PAGE_SIZE = 128

HG_HEADS = 4
HG_DK = 128
HG_DV = 128
HG_WIDTH = HG_HEADS * HG_DV
HG_CHUNK = 64
AT_HEADS = 4
AT_DH = 128
AT_WIDTH = AT_HEADS * AT_DH
IDX_HEADS = 8
IDX_DIM = 64
IDX_SCALE = (IDX_HEADS * IDX_DIM) ** -0.5
TOPK_MAX = 256
Q_BLOCK = 128
D_FF = 4 * D_MODEL
EPS = 1e-6

_WIDTHS = (HG_HEADS * HG_DK, HG_HEADS * HG_DK, HG_WIDTH, HG_WIDTH,
           AT_WIDTH, AT_WIDTH, AT_WIDTH,
           IDX_HEADS * IDX_DIM, IDX_DIM, IDX_HEADS,
           D_MODEL, D_MODEL)
D_IN = sum(_WIDTHS)

kernel_name = "hgrn2_dsa_gated_hybrid_step"


def _rmsnorm(x, g):
    xf = x.astype(jnp.float32)
    y = xf * lax.rsqrt(jnp.mean(xf * xf, axis=-1, keepdims=True) + EPS) * g.astype(jnp.float32)
    return y.astype(x.dtype)


def _hgrn2(q, k, v, logf, s0):
    f32 = jnp.float32
    B, T, H, DK = q.shape
    DV = v.shape[-1]
    c = HG_CHUNK if T % HG_CHUNK == 0 else T
    n = T // c

    def blocks(t):
        return t.astype(f32).reshape(B, n, c, H, t.shape[-1]).transpose(1, 0, 3, 2, 4)

    tri = jnp.tril(jnp.ones((c, c), dtype=bool))

    def step(S, inp):
        qc, kc, vc, lc = inp
        b = jnp.cumsum(lc, axis=2)
        o = jnp.einsum('bhtd,bhde->bhte', qc * jnp.exp(b), S)
        diff = jnp.where(tri[:, :, None], b[:, :, :, None, :] - b[:, :, None, :, :], -jnp.inf)
        a = jnp.sum(qc[:, :, :, None, :] * kc[:, :, None, :, :] * jnp.exp(diff), axis=-1)
        o = o + jnp.einsum('bhts,bhse->bhte', a, vc)
        b_end = b[:, :, -1:, :]
        S = jnp.exp(b_end[:, :, 0, :, None]) * S + jnp.einsum('bhsd,bhse->bhde', kc * jnp.exp(b_end - b), vc)
        return S, o

    S, o = lax.scan(step, s0.astype(f32), (blocks(q), blocks(k), blocks(v), blocks(logf)))
    o = o.transpose(1, 0, 3, 2, 4).reshape(B, T, H, DV)
    return o.astype(q.dtype), S.astype(s0.dtype)


def _sparse_attend(q, qi, w, qpos, k, v, kidx, n_sel):
    f32 = jnp.float32
    L = k.shape[1]
    dots = jnp.einsum('bqhd,bld->bqhl', qi.astype(f32), kidx.astype(f32))
    score = jnp.einsum('bqhl,bqh->bql', jax.nn.relu(dots), w.astype(f32)) * IDX_SCALE
    allowed = jnp.arange(L)[None, :] <= qpos[:, None]
    score = jnp.where(allowed[None], score, -jnp.inf)
    _, sel = lax.top_k(score, n_sel)
    valid = sel <= qpos[None, :, None]
    take = jax.vmap(lambda t, i: t[i])
    kg = take(k, sel).astype(f32)
    vg = take(v, sel).astype(f32)
    logits = jnp.einsum('bqhd,bqkhd->bqhk', q.astype(f32), kg) * (AT_DH ** -0.5)
    logits = jnp.where(valid[:, :, None, :], logits, -jnp.inf)
    p = jax.nn.softmax(logits, axis=-1)
    return jnp.einsum('bqhk,bqkhd->bqhd', p, vg).astype(q.dtype)


def _attend_prompt(q, k, v, qi, kidx, w):
    B, T = q.shape[0], q.shape[1]
    n_sel = min(TOPK_MAX, T // 4)
    nb = T // Q_BLOCK

    def to_blocks(t):
        return t.reshape(B, nb, Q_BLOCK, *t.shape[2:]).swapaxes(0, 1)

    def blk(args):
        qb, qib, wb, qpos = args
        return _sparse_attend(qb, qib, wb, qpos, k, v, kidx, n_sel)

    qpos = jnp.arange(T).reshape(nb, Q_BLOCK)
    out = lax.map(blk, (to_blocks(q), to_blocks(qi), to_blocks(w), qpos))
    return out.swapaxes(0, 1).reshape(B, T, AT_HEADS, AT_DH)


def _attend_sample(q, k, v, qi, kidx, w, past_k, past_v, past_kidx):
    T = q.shape[1]
    P = past_k.shape[1]
    kf = jnp.concatenate([past_k, k.astype(past_k.dtype)], axis=1)
    vf = jnp.concatenate([past_v, v.astype(past_v.dtype)], axis=1)
    kif = jnp.concatenate([past_kidx, kidx.astype(past_kidx.dtype)], axis=1)
    n_sel = min(TOPK_MAX, (P + T) // 4)
    qpos = P + jnp.arange(T)
    return _sparse_attend(q, qi, w, qpos, kf, vf, kif, n_sel)


def _layer(x, s0, attend, lb, w_in, hg_norm, w_a, w_b, w_o, norm1, norm2, w_up, w_down):
    B, T, _ = x.shape
    h = _rmsnorm(x, norm1)
    z = h @ w_in
    offs = np.cumsum(_WIDTHS)[:-1].tolist()
    hq, hf, hi, hog, aq, ak, av, iq, ik, iw, ga, gb = jnp.split(z, offs, axis=-1)

    lbf = lb.astype(jnp.float32)
    logf = jnp.logaddexp(jnp.log(lbf), jnp.log1p(-lbf) + jax.nn.log_sigmoid(hf.astype(jnp.float32)))
    kk = -jnp.expm1(logf)
    hd = lambda t, d: t.reshape(B, T, HG_HEADS, d)
    o, s_new = _hgrn2(hd(hq, HG_DK), hd(kk, HG_DK), hd(hi, HG_DV), hd(logf, HG_DK), s0)
    o = _rmsnorm(o, hg_norm) * jax.nn.silu(hd(hog, HG_DV))
    y_a = o.reshape(B, T, HG_WIDTH) @ w_a

    ak = ak.reshape(B, T, AT_HEADS, AT_DH)
    av = av.reshape(B, T, AT_HEADS, AT_DH)
    ao = attend(aq.reshape(B, T, AT_HEADS, AT_DH), ak, av,
                iq.reshape(B, T, IDX_HEADS, IDX_DIM), ik, iw)
    y_b = ao.reshape(B, T, AT_WIDTH) @ w_b

    m = jax.nn.sigmoid(ga) * y_a + jax.nn.sigmoid(gb) * y_b
    x = x + m @ w_o
    h2 = _rmsnorm(x, norm2)
    x = x + jnp.square(jax.nn.relu(h2 @ w_up)) @ w_down
    return x, s_new, ak, av, ik


def setup_inputs(seed: int = 0) -> dict:
    key = jax.random.key(seed)
    ks = jax.random.split(key, 24)
    n_pages = PAST_LEN // PAGE_SIZE
    n_used = DEC_BATCH * n_pages
    n_phys = n_used + n_used // 4
    nrm = lambda k, shape, s: jax.random.normal(k, shape, jnp.float32) * s
    page_table = jax.random.permutation(ks[0], n_phys)[:n_used].reshape(DEC_BATCH, n_pages).astype(jnp.int32)
    return {
        "x_prompt": nrm(ks[1], (BATCH, SEQ, D_MODEL), 1.0),
        "x_sample": nrm(ks[2], (DEC_BATCH, DEC_SEQ, D_MODEL), 1.0),
        "cache_k": nrm(ks[3], (DEPTH, n_phys, PAGE_SIZE, AT_HEADS, AT_DH), 1.0),
        "cache_v": nrm(ks[4], (DEPTH, n_phys, PAGE_SIZE, AT_HEADS, AT_DH), 1.0),
        "cache_idx_k": nrm(ks[5], (DEPTH, n_phys, PAGE_SIZE, IDX_DIM), 1.0),
        "state_hgrn": nrm(ks[6], (DEPTH, DEC_BATCH, HG_HEADS, HG_DK, HG_DV), 0.3),
        "page_table": page_table,
        "lb_logits": nrm(ks[7], (DEPTH + 1, HG_HEADS * HG_DK), 0.5),
        "w_in": nrm(ks[8], (DEPTH, D_MODEL, D_IN), D_MODEL ** -0.5),
        "hg_norm": 1.0 + nrm(ks[9], (DEPTH, HG_DV), 0.01),
        "w_a": nrm(ks[10], (DEPTH, HG_WIDTH, D_MODEL), HG_WIDTH ** -0.5),
        "w_b": nrm(ks[11], (DEPTH, AT_WIDTH, D_MODEL), AT_WIDTH ** -0.5),
        "w_o": nrm(ks[12], (DEPTH, D_MODEL, D_MODEL), D_MODEL ** -0.5),
        "norm1": 1.0 + nrm(ks[13], (DEPTH, D_MODEL), 0.01),
        "norm2": 1.0 + nrm(ks[14], (DEPTH, D_MODEL), 0.01),
        "w_up": nrm(ks[15], (DEPTH, D_MODEL, D_FF), D_MODEL ** -0.5),
        "w_down": nrm(ks[16], (DEPTH, D_FF, D_MODEL), D_FF ** -0.5),
        "norm_f": 1.0 + nrm(ks[17], (D_MODEL,), 0.01),
    }


def reference(x_prompt, x_sample, cache_k, cache_v, cache_idx_k, state_hgrn, page_table,
              lb_logits, w_in, hg_norm, w_a, w_b, w_o, norm1, norm2, w_up, w_down, norm_f):
    lb_all = jnp.cumsum(jax.nn.softmax(lb_logits.astype(jnp.float32), axis=0), axis=0)
    n_seq = page_table.shape[0]
    xp, xs = x_prompt, x_sample
    kp_l, vp_l, ip_l, sp_l, ks_l, vs_l, is_l, ss_l = [], [], [], [], [], [], [], []
    for l in range(DEPTH):
        prm = (lb_all[l], w_in[l], hg_norm[l], w_a[l], w_b[l], w_o[l], norm1[l], norm2[l], w_up[l], w_down[l])
        s0 = jnp.zeros((xp.shape[0], HG_HEADS, HG_DK, HG_DV), state_hgrn.dtype)
        xp, sp, kp, vp, ip = _layer(xp, s0, _attend_prompt, *prm)
        gather = lambda c: c[l][page_table].reshape(n_seq, -1, *c.shape[3:])
        attend_s = functools.partial(_attend_sample, past_k=gather(cache_k), past_v=gather(cache_v),
                                     past_kidx=gather(cache_idx_k))
        xs, ss, ks_, vs, is_ = _layer(xs, state_hgrn[l], attend_s, *prm)
        kp_l.append(kp); vp_l.append(vp); ip_l.append(ip); sp_l.append(sp)
        ks_l.append(ks_); vs_l.append(vs); is_l.append(is_); ss_l.append(ss)
    y_prompt = _rmsnorm(xp, norm_f)
    y_sample = _rmsnorm(xs, norm_f)
    return (y_prompt, y_sample,
            jnp.stack(kp_l), jnp.stack(vp_l), jnp.stack(ip_l), jnp.stack(sp_l),
            jnp.stack(ks_l), jnp.stack(vs_l), jnp.stack(is_l), jnp.stack(ss_l))
```

```python
import os
import numpy as np
from contextlib import ExitStack
import concourse.bass as bass
import concourse.mybir as mybir
from concourse.bass_utils import run_bass_kernel_spmd

F32 = mybir.dt.float32
BF16 = mybir.dt.bfloat16
I32 = mybir.dt.int32
AF = mybir.ActivationFunctionType
ALU = mybir.AluOpType
AX = mybir.AxisListType

ENGS = ('pe', 'act', 'dve', 'pool', 'sp')
_ENV = os.environ if os.environ.get('MK_DEBUG') else {}


class Buf:
    def __init__(self, name, t):
        self.name = name
        self.t = t
        self.w = None
        self.r = []
        self.dcnt = 0
        self.stores = []


class Prog:
    def __init__(self, nc, es):
        self.nc = nc
        self.es = es
        self.ops = {e: [] for e in ENGS}
        self.cnt = {e: 0 for e in ENGS}
        self.seen = {e: {} for e in ENGS}
        self.dsems = {}
        self.nbuf = 0
        self.cbuf = None
        self.cvals = {}

    def sb(self, name, shape, dtype):
        t = self.es.enter_context(self.nc.sbuf_tensor(name, list(shape), dtype))
        return Buf(name, t)

    def ps(self, name, shape, dtype):
        t = self.es.enter_context(self.nc.psum_tensor(name, list(shape), dtype))
        return Buf(name, t)

    def view(self, name, t):
        return Buf(name, t)

    def const_col(self, val):
        if self.cbuf is None:
            self.cbuf = self.sb("constcols", [128, 32], F32)
        if val not in self.cvals:
            i = len(self.cvals)
            assert i < 32
            self.cvals[val] = i
            self.op('pool', lambda e, i=i, val=val: e.memset(self.cbuf.t[:, i:i + 1], float(val)), r=[], w=[self.cbuf])
        i = self.cvals[val]
        return self.cbuf.t[:, i:i + 1]

    def _deps(self, eng, r, w):
        deps = {}
        def add(tok):
            if tok is None:
                return
            k, v = tok
            if k == eng and eng == 'pe':
                return
            if deps.get(k, 0) < v:
                deps[k] = v
        for b in r:
            add(b.w)
        for b in w:
            add(b.w)
            for tok in b.r:
                add(tok)
        waits = []
        seen = self.seen[eng]
        for k, v in deps.items():
            if seen.get(k, 0) >= v:
                continue
            seen[k] = v
            waits.append((k, v))
        return waits

    def _commit(self, tok, r, w):
        ws = set(id(b) for b in w)
        for b in w:
            b.w = tok
            b.r = []
        for b in r:
            if id(b) not in ws:
                b.r.append(tok)

    def op(self, eng, fn, r, w):
        if self.cbuf is not None and self.cbuf not in r and self.cbuf not in w:
            r = list(r) + [self.cbuf]
        waits = self._deps(eng, r, w)
        self.cnt[eng] += 1
        tok = (eng, self.cnt[eng])
        self.ops[eng].append((waits, fn, (eng, 1)))
        self._commit(tok, r, w)

    def dma(self, q, out_ap, in_ap, r, w, indirect=None, sem=None, dw=None, dr=None):
        sb = sem if sem is not None else (w[0] if w else r[0])
        waits = self._deps(q, r, w)
        if dr is not None:
            seen = self.seen[q]
            for (k, v) in dr.stores:
                if seen.get(k, 0) < v:
                    seen[k] = v
                    waits = [x for x in waits if x[0] != k] + [(k, v)]
        sb.dcnt += 1
        key = ('d', sb.name)
        self.dsems[sb.name] = sb
        tok = (key, 16 * sb.dcnt)
        if indirect is None:
            fn = lambda e: e.dma_start(out=out_ap, in_=in_ap)
        else:
            fn = lambda e: e.indirect_dma_start(out=out_ap, out_offset=None, in_=in_ap,
                                                in_offset=bass.IndirectOffsetOnAxis(ap=indirect, axis=0))
        self.ops[q].append((waits, fn, (key, 16)))
        self._commit(tok, r, w)
        if dw is not None:
            dw.stores.append(tok)

    def finish(self):
        nc = self.nc
        sem = {}
        for e in ENGS:
            sem[e] = self.es.enter_context(nc.semaphore("s_" + e))
        for name in self.dsems:
            sem[('d', name)] = self.es.enter_context(nc.semaphore("d_" + name))
        final = [(('d', n), 16 * b.dcnt) for n, b in self.dsems.items()]
        final += [(e, self.cnt[e]) for e in ENGS if e != 'sp' and self.cnt[e] > 0]

        def replay(ename, e):
            for waits, fn, inc in self.ops[ename]:
                for k, v in waits:
                    e.wait_ge(sem[k], v)
                ins = fn(e)
                ins.then_inc(sem[inc[0]], inc[1])
            if ename == 'sp':
                for k, v in final:
                    e.wait_ge(sem[k], v)

        with nc.Block() as block:
            @block.tensor
            def _(e):
                replay('pe', e)

            @block.scalar
            def _(e):
                replay('act', e)

            @block.vector
            def _(e):
                replay('dve', e)

            @block.gpsimd
            def _(e):
                replay('pool', e)

            @block.sync
            def _(e):
                replay('sp', e)


C_HF, C_HI, C_AV, C_AK, C_IK, C_HQ, C_HOG, C_AQ, C_IQ, C_IW, C_GA, C_GB, C_END = (
    0, 512, 1024, 1536, 2048, 2112, 2624, 3136, 3648, 4160, 4168, 5192, 6216)
R_OFF = dict(hq=0, hf=512, hi=1024, hog=1536, aq=2048, ak=2560, av=3072, iq=3584, ik=4096, iw=4160,
             ga=4168, gb=5192)
BIG = 1.0e30
EPS = 1e-6
K_ID, K_U64, K_L64, K_U4, K_L4, K_CNEG, K_IND64, K_IND4, K_ONES, K_END = (
    0, 128, 256, 384, 512, 640, 768, 770, 802, 930)


def make_consts():
    c = np.zeros((128, 1024), np.float32)
    p = np.arange(128)[:, None]
    j = np.arange(128)[None, :]
    c[:, K_ID:K_ID + 128] = (p == j)
    for (ku, kl, C) in ((K_U64, K_L64, 64), (K_U4, K_L4, 4)):
        same = (p // C) == (j // C)
        c[:, ku:ku + 128] = same & (p <= j)
        c[:, kl:kl + 128] = same & (p > j)
    c[:, K_CNEG:K_CNEG + 128] = np.where(j <= p, 0.0, -BIG)
    c[:, K_IND64:K_IND64 + 2] = (p // 64) == np.arange(2)[None, :]
    c[:, K_IND4:K_IND4 + 32] = (p // 4) == np.arange(32)[None, :]
    c[:, K_ONES:K_ONES + 128] = 1.0
    return c


A_HF, A_HI, A_AV, A_AK, A_IK, A_HQ, A_HOG, A_GA, A_END = 0, 512, 1024, 1536, 2048, 2112, 2624, 3136, 4160
B_AQ, B_IQ, B_IW, B_GB, B_END = 0, 512, 1024, 1032, 2056
ARENA = 8 * A_END + 4 * 1024


def build(cfg):
    NT, NSEQ, NPG, NPHYS = cfg['NT'], cfg['NSEQ'], cfg['NPG'], cfg['NPHYS']
    KSEL_P, KSEL_S = cfg['KSEL_P'], cfg['KSEL_S']
    NIT = cfg.get('NIT', 18)
    NOWN = NT // 4
    LP = NT * 128
    LS = (NPG + 1) * 128
    LMAX = max(LP, LS)
    NCH = LMAX // 512 + 1
    nc = bass.Bass("TRN2", target_bir_lowering=False)

    def din(name, shape, dt=F32):
        return nc.dram_tensor(name, list(shape), dt, kind="ExternalInput").ap()

    def dout(name, shape, dt=F32):
        return nc.dram_tensor(name, list(shape), dt, kind="ExternalOutput").ap()

    def dscr(name, shape, dt):
        return nc.dram_tensor(name, list(shape), dt, kind="Internal").ap()

    xp = din("xp", [LP, 1024]); xs = din("xs", [128, 1024]); padb = din("padb", [128, 4])
    st0 = din("st0", [NSEQ * 4 * 128, 128]); ptab = din("ptab", [1, NSEQ * NPG], I32)
    ck = din("ck", [NPHYS * 128, 512]); cv = din("cv", [NPHYS * 128, 512]); cik = din("cik", [NPHYS * 128, 64])
    cst = din("cst", [128, 1024]); w1 = din("w1", [1024, A_END]); w2 = din("w2", [1024, B_END])
    w_a = din("w_a", [512, 1024]); w_b = din("w_b", [512, 1024]); w_o = din("w_o", [1024, 1024])
    w_up = din("w_up", [1024, 4096]); w_dn = din("w_dn", [4096, 1024])
    lbl = din("lbl", [2, 512]); hgn = din("hgn", [128, 1]); n1 = din("n1", [1, 1024]); n2 = din("n2", [1, 1024])
    nf = din("nf", [1, 1024])
    yp = dout("yp", [NOWN * 128, 1024]); kp = dout("kp", [NOWN * 128, 512]); vp = dout("vp", [NOWN * 128, 512])
    ikp = dout("ikp", [NOWN * 128, 64]); stp = dout("stp", [4 * 128, 128])
    ys = dout("ys", [128, 1024]); ks = dout("ks", [128, 512]); vs = dout("vs", [128, 512])
    iks = dout("iks", [128, 64]); sts = dout("sts", [NSEQ * 4 * 128, 128])
    kt_scr = dscr("kt_scr", [4, 128, LP], BF16)
    v_scr = dscr("v_scr", [LP, 512], BF16)
    ma_scr = dscr("ma_scr", [(NOWN + 1) * 128, 1024], F32)
    x1_scr = dscr("x1_scr", [(NOWN + 1) * 128, 1024], F32)

    es = ExitStack()
    P = Prog(nc, es)
    op, dma = P.op, P.dma

    W = P.sb("arena", [128, ARENA], BF16)
    U1 = P.sb("u1", [128, 8192], F32)
    JK = P.sb("junk8", [128, 8192], mybir.dt.uint8)
    CF = P.sb("cf", [128, 1024], F32); CB = P.sb("cb", [128, 1024], BF16)
    GG = es.enter_context(nc.sbuf_tensor("gg", [128, 2048], F32))
    G1 = Buf("g1", GG[:, 0:1024]); LB = Buf("lb", GG[:, 1024:1536]); OML = Buf("oml", GG[:, 1536:2048])
    G2 = Buf("g2", GG[:, 0:1024]); GF = Buf("gf", GG[:, 1024:2048])
    HG = P.sb("hg", [128, 1], F32)
    PADB = P.sb("padbs", [128, 4], F32); SS = P.sb("ss", [128, 8], F32)
    XT = [P.sb("xt%d" % i, [128, 1024], F32) for i in range(2)]
    H = P.sb("h", [128, 1024], BF16); HT = P.sb("hT", [128, 8, 128], BF16)
    NSCR = 16 * 1024
    SCR = es.enter_context(nc.sbuf_tensor("scr", [128, NSCR], F32))
    cur = dict(off=0, bufs=[])

    def carve(name, shape, dt):
        n = int(np.prod(shape[1:]))
        nf32 = (n + 1) // 2 if dt == BF16 else n
        nf32 = (nf32 + 7) // 8 * 8
        o = cur['off']
        assert o + nf32 <= NSCR, (name, o, nf32)
        cur['off'] = o + nf32
        ap = SCR[0:shape[0], o:o + nf32]
        if dt == BF16:
            ap = ap.bitcast(BF16)[:, 0:n]
        else:
            ap = ap[:, 0:n]
        if dt == I32:
            ap = SCR[0:shape[0], o:o + nf32].bitcast(I32)[:, 0:n]
        if len(shape) == 3:
            ap = ap.rearrange("p (a b) -> p a b", a=shape[1])
        b = Buf(name, ap)
        cur['bufs'].append(b)
        return b

    def new_pass():
        old = cur['bufs']
        cur['off'] = 0
        cur['bufs'] = []
        return old

    def fence(bufs):
        if bufs:
            op('pool', lambda e: e.memset(SS.t[:, 7:8], 0.0), r=[], w=list(bufs) + [SS])

    PB = [P.ps("pb%d" % i, [128, 512], F32) for i in range(6)]
    PTR = [P.ps("ptr%d" % i, [128, 8, 128], BF16) for i in range(2)]
    st = dict(bank=0, banks=list(range(6)), tr=0, xi=0)

    def bank():
        b = PB[st['banks'][st['bank'] % len(st['banks'])]]
        st['bank'] += 1
        return b

    def trbank():
        b = PTR[st['tr'] % 2]
        st['tr'] += 1
        return b

    IDB = CB.t[:, K_ID:K_ID + 128]

    dma('sp', CF.t[:, :], cst, r=[], w=[CF])
    op('dve', lambda e: e.tensor_copy(out=CB.t[:, :], in_=CF.t[:, :]), r=[CF], w=[CB])
    dma('sp', G1.t[:, :], n1.partition_broadcast(128), r=[], w=[G1])
    dma('sp', LB.t[:, :], lbl[0:1, :].partition_broadcast(128), r=[], w=[LB])
    dma('sp', OML.t[:, :], lbl[1:2, :].partition_broadcast(128), r=[], w=[OML])
    dma('sp', HG.t[:, :], hgn, r=[], w=[HG])
    dma('sp', PADB.t[:, :], padb, r=[], w=[PADB])
    op('dve', lambda e: e.tensor_tensor(out=LB.t[:, :], in0=LB.t[:, :], in1=OML.t[:, :], op=ALU.subtract), r=[LB, OML], w=[LB])
    op('act', lambda e: e.activation(out=LB.t[:, :], in_=LB.t[:, :], func=AF.Sigmoid), r=[LB], w=[LB])
    op('dve', lambda e: e.tensor_scalar(out=OML.t[:, :], in0=LB.t[:, :], scalar1=-1.0, scalar2=1.0, op0=ALU.mult, op1=ALU.add), r=[LB], w=[OML])

    EPSC = P.const_col(EPS)
    WCOLS = dict(n=A_END)

    def Wc(k, off, n):
        return W.t[:, k * WCOLS['n'] + off:k * WCOLS['n'] + off + n]

    def load_w(src, ncols, extra):
        WCOLS['n'] = ncols
        for k in range(8):
            for c0 in range(0, ncols, 2048):
                c1 = min(ncols, c0 + 2048)
                dma('pool', W.t[:, k * ncols + c0:k * ncols + c1], src[k * 128:(k + 1) * 128, c0:c1], r=[], w=[W])
        o = 8 * ncols
        outs = []
        for (ap, nch) in extra:
            for k in range(nch):
                dma('pool', W.t[:, o + k * 1024:o + (k + 1) * 1024], ap[k * 128:(k + 1) * 128, :], r=[], w=[W])
            outs.append(o)
            o += nch * 1024
        assert o <= ARENA
        return outs

    def proj_tok(off, n):
        b = bank()
        for k in range(8):
            op('pe', lambda e, k=k, wap=Wc(k, off, n): e.matmul(b.t[:, 0:n], lhsT=HT.t[:, k, :], rhs=wap, start=(k == 0), stop=(k == 7)),
               r=[HT, W], w=[b])
        return b

    def proj_feat(off, n, b, c0):
        for k in range(8):
            op('pe', lambda e, k=k, wap=Wc(k, off, n): e.matmul(b.t[0:n, c0:c0 + 128], lhsT=wap, rhs=HT.t[:, k, :], start=(k == 0), stop=(k == 7)),
               r=[HT, W], w=[b])

    def rmsnorm_rstd(X, D=1024.0):
        op('dve', lambda e: e.scalar_tensor_tensor(out=JK.t[:, 0:1024], in0=X.t[:, :], scalar=1.0, in1=X.t[:, :], op0=ALU.mult, op1=ALU.mult,
                                                   accum_out=SS.t[:, 0:1]), r=[X], w=[JK, SS])
        op('dve', lambda e: e.tensor_copy(out=SS.t[:, 1:2], in_=SS.t[:, 0:1]), r=[SS], w=[SS])
        op('act', lambda e: e.activation(out=SS.t[:, 2:3], in_=SS.t[:, 1:2], func=AF.Sqrt, scale=1.0 / D,
                                         bias=EPSC), r=[SS], w=[SS])
        op('dve', lambda e: e.reciprocal(out=SS.t[:, 3:4], in_=SS.t[:, 2:3]), r=[SS], w=[SS])
        return SS.t[:, 3:4]

    def to_hT(X, G):
        rstd = rmsnorm_rstd(X)
        op('dve', lambda e: e.scalar_tensor_tensor(out=H.t[:, :], in0=X.t[:, :], scalar=rstd, in1=G.t[:, :], op0=ALU.mult, op1=ALU.mult),
           r=[X, SS, G], w=[H])
        tb = trbank()
        for k in range(8):
            op('pe', lambda e, k=k: e.transpose(out=tb.t[:, k, :], in_=H.t[:, k * 128:(k + 1) * 128], identity=IDB), r=[H, CB], w=[tb])
        op('act', lambda e: e.copy(out=HT.t[:, :, :], in_=tb.t[:, :, :]), r=[tb], w=[HT])

    def load_x(src_ap):
        X = XT[st['xi'] % 2]
        st['xi'] += 1
        dma('sp', X.t[:, :], src_ap, r=[], w=[X])
        return X

    D_V, D_KT, D_IKT, D_MA, D_X1, D_S = (Buf(n, None) for n in ("D_V", "D_KT", "D_IKT", "D_MA", "D_X1", "D_S"))
    ikt_scr = dscr("ikt_scr", [64, LP], BF16)
    kts_scr = dscr("kts_scr", [4, 128, 128], BF16)
    vs_scr = dscr("vs_scr", [128, 512], BF16)
    iks_scr = dscr("iks_scr", [64, 128], BF16)

    woff = load_w(w1, A_END, [(w_a, 4)])
    WA0 = woff[0]
    TP = [carve("tp%d" % i, [128, 512], F32) for i in range(6)]
    tpi = dict(i=0)

    def tmp():
        b = TP[tpi['i'] % len(TP)]
        tpi['i'] += 1
        return b

    LOGF = carve("logf", [128, 512], F32); KK = carve("kk", [128, 512], F32)
    VB = carve("vb", [128, 512], BF16); VA = carve("va", [128, 512], BF16)
    KHAT = carve("khat", [128, 512], BF16); QTL = carve("qtl", [128, 512], BF16); KTL = carve("ktl", [128, 512], BF16)
    QTT = carve("qtT", [128, 4, 128], BF16); KTT = carve("ktT", [128, 4, 128], BF16)
    AT = carve("at", [128, 4, 128], BF16); DEC = carve("dec", [128, 64], F32)
    SF = carve("sf", [128, 4, 128], F32); SBF = [carve("sbf%d" % i, [128, 4, 128], BF16) for i in range(3)]
    SQ = carve("sq", [128, 512], BF16); OF = carve("of", [128, 4, 128], BF16)
    KHC = [carve("khc%d" % i, [128, 512], BF16) for i in range(2)]
    KTILE = carve("ktile", [128, 4, 128], BF16); IKTILE = carve("iktile", [64, 128], BF16)
    IKF = carve("ikf", [128, 64], F32)
    S0F = [carve("s0f%d" % i, [128, 4, 128], F32) for i in range(2)]
    S0B = [carve("s0b%d" % i, [128, 4, 128], BF16) for i in range(2)]
    VM = [carve("vm%d" % i, [128, 512], BF16) for i in range(2)]
    SNEW = [carve("snew%d" % i, [128, 4, 128], F32) for i in range(2)]
    for i in range(3):
        op('pool', lambda e, i=i: e.memset(SBF[i].t[:, :, :], 0.0), r=[], w=[SBF[i]])
    op('pool', lambda e: e.memset(SF.t[:, :, :], 0.0), r=[], w=[SF])
    sidx = dict(i=0)

    CUT = int(_ENV.get("CUT", "0"))

    def pass1_tile(X, kind, j, oi):
        own = kind != 'light'
        samp = kind == 'samp'
        KU, KL, KIND, NCK = (K_U4, K_L4, K_IND4, NSEQ) if samp else (K_U64, K_L64, K_IND64, 2)
        to_hT(X, G1)
        b = proj_tok(A_HF, 512)
        sig = tmp()
        op('act', lambda e: e.activation(out=sig.t[:, :], in_=b.t[:, :], func=AF.Sigmoid), r=[b], w=[sig])
        f = tmp()
        op('pool', lambda e: e.tensor_tensor(out=f.t[:, :], in0=sig.t[:, :], in1=OML.t[:, :], op=ALU.mult), r=[sig, OML], w=[f])
        op('pool', lambda e: e.tensor_tensor(out=f.t[:, :], in0=f.t[:, :], in1=LB.t[:, :], op=ALU.add), r=[f, LB], w=[f])
        op('act', lambda e: e.activation(out=LOGF.t[:, :], in_=f.t[:, :], func=AF.Ln), r=[f], w=[LOGF])
        op('dve', lambda e: e.tensor_scalar(out=KK.t[:, :], in0=f.t[:, :], scalar1=-1.0, scalar2=1.0, op0=ALU.mult, op1=ALU.add), r=[f], w=[KK])
        if CUT == 1:
            return
        b2 = proj_tok(A_HI, 512)
        op('act', lambda e: e.copy(out=VB.t[:, :], in_=b2.t[:, :]), r=[b2], w=[VB])
        b3 = proj_tok(A_AV, 512)
        op('act', lambda e: e.copy(out=VA.t[:, :], in_=b3.t[:, :]), r=[b3], w=[VA])
        if samp:
            dma('sp', vs_scr, VA.t[:, :], r=[VA], w=[], dw=D_S)
        else:
            dma('sp', v_scr[j * 128:(j + 1) * 128, :], VA.t[:, :], r=[VA], w=[], dw=D_V)
        if own:
            t = tmp()
            op('dve', lambda e, t=t: e.tensor_copy(out=t.t[:, :], in_=b3.t[:, :]), r=[b3], w=[t])
            dma('sp', vs if samp else vp[oi * 128:(oi + 1) * 128, :], t.t[:, :], r=[t], w=[])
        if CUT == 2:
            return
        kb = bank()
        for h in range(4):
            proj_feat(A_AK + h * 128, 128, kb, h * 128)
        op('act', lambda e: e.copy(out=KTILE.t[:, :, :], in_=kb.t[:, :].rearrange("p (h t) -> p h t", h=4)), r=[kb], w=[KTILE])
        if samp:
            dma('sp', kts_scr.rearrange("h d t -> d h t"), KTILE.t[:, :, :], r=[KTILE], w=[], dw=D_S)
        else:
            dma('sp', kt_scr[:, :, j * 128:(j + 1) * 128].rearrange("h d t -> d h t"), KTILE.t[:, :, :], r=[KTILE], w=[], dw=D_KT)
        ib = bank()
        proj_feat(A_IK, 64, ib, 0)
        op('act', lambda e: e.copy(out=IKTILE.t[:, :], in_=ib.t[0:64, 0:128]), r=[ib], w=[IKTILE])
        dma('sp', iks_scr if samp else ikt_scr[:, j * 128:(j + 1) * 128], IKTILE.t[:, :], r=[IKTILE], w=[], dw=(D_S if samp else D_IKT))
        if own:
            b4 = proj_tok(A_AK, 512)
            t = tmp()
            op('dve', lambda e, t=t: e.tensor_copy(out=t.t[:, :], in_=b4.t[:, :]), r=[b4], w=[t])
            dma('sp', ks if samp else kp[oi * 128:(oi + 1) * 128, :], t.t[:, :], r=[t], w=[])
            b5 = proj_tok(A_IK, 64)
            op('dve', lambda e: e.tensor_copy(out=IKF.t[:, :], in_=b5.t[:, 0:64]), r=[b5], w=[IKF])
            dma('sp', iks if samp else ikp[oi * 128:(oi + 1) * 128, :], IKF.t[:, :], r=[IKF], w=[])
        if CUT == 3:
            return
        bd = bank()
        op('pe', lambda e: e.matmul(bd.t[:, :], lhsT=CF.t[:, KL:KL + 128], rhs=LOGF.t[:, :], start=True, stop=True), r=[CF, LOGF], w=[bd])
        ebd = tmp()
        op('act', lambda e: e.activation(out=ebd.t[:, :], in_=bd.t[:, :], func=AF.Exp), r=[bd], w=[ebd])
        op('pool', lambda e: e.tensor_tensor(out=KHAT.t[:, :], in0=KK.t[:, :], in1=ebd.t[:, :], op=ALU.mult), r=[KK, ebd], w=[KHAT])
        dc = bank()
        for h in range(4):
            op('pe', lambda e, h=h: e.matmul(dc.t[:, h * NCK:(h + 1) * NCK], lhsT=LOGF.t[:, h * 128:(h + 1) * 128],
                                             rhs=CF.t[:, KIND:KIND + NCK], start=True, stop=True), r=[CF, LOGF], w=[dc])
        op('act', lambda e: e.activation(out=DEC.t[:, 0:4 * NCK], in_=dc.t[:, 0:4 * NCK], func=AF.Exp), r=[dc], w=[DEC])
        if CUT == 11:
            return
        if own:
            bt = bank()
            op('pe', lambda e: e.matmul(bt.t[:, :], lhsT=CF.t[:, KU:KU + 128], rhs=LOGF.t[:, :], start=True, stop=True), r=[CF, LOGF], w=[bt])
            eb = tmp(); enb = tmp()
            op('act', lambda e: e.activation(out=eb.t[:, :], in_=bt.t[:, :], func=AF.Exp), r=[bt], w=[eb])
            op('act', lambda e: e.activation(out=enb.t[:, :], in_=bt.t[:, :], func=AF.Exp, scale=-1.0), r=[bt], w=[enb])
            if CUT == 12:
                return
            hq = proj_tok(A_HQ, 512)
            op('dve', lambda e: e.tensor_tensor(out=QTL.t[:, :], in0=hq.t[:, :], in1=eb.t[:, :], op=ALU.mult), r=[hq, eb], w=[QTL])
            op('pool', lambda e: e.tensor_tensor(out=KTL.t[:, :], in0=KK.t[:, :], in1=enb.t[:, :], op=ALU.mult), r=[KK, enb], w=[KTL])
            if CUT == 13:
                return
            tb = trbank()
            for h in range(4):
                op('pe', lambda e, h=h: e.transpose(out=tb.t[:, h, :], in_=QTL.t[:, h * 128:(h + 1) * 128], identity=IDB), r=[QTL, CB], w=[tb])
            for h in range(4):
                op('pe', lambda e, h=h: e.transpose(out=tb.t[:, 4 + h, :], in_=KTL.t[:, h * 128:(h + 1) * 128], identity=IDB), r=[KTL, CB], w=[tb])
            if CUT == 14:
                return
            op('act', lambda e: e.copy(out=QTT.t[:, :, :], in_=tb.t[:, 0:4, :]), r=[tb], w=[QTT])
            if CUT == 15:
                return
            op('act', lambda e: e.copy(out=KTT.t[:, :, :], in_=tb.t[:, 4:8, :]), r=[tb], w=[KTT])
            if CUT == 7:
                return
            ab = bank()
            for h in range(4):
                op('pe', lambda e, h=h: e.matmul(ab.t[:, h * 128:(h + 1) * 128], lhsT=KTT.t[:, h, :], rhs=QTT.t[:, h, :], start=True, stop=True),
                   r=[KTT, QTT], w=[ab])
            for h in range(4):
                op('dve', lambda e, h=h: e.tensor_tensor(out=AT.t[:, h, :], in0=ab.t[:, h * 128:(h + 1) * 128], in1=CF.t[:, KU:KU + 128], op=ALU.mult),
                   r=[ab, CF], w=[AT])
        if CUT == 4:
            return
        obh = None
        if not samp:
            si = sidx['i']
            s_start, s_mid, s_end = SBF[si % 3], SBF[(si + 1) % 3], SBF[(si + 2) % 3]
            sidx['i'] += 2
            for c, s_out in ((0, s_mid), (1, s_end)):
                op('pool', lambda e, c=c: e.tensor_scalar(out=KHC[c].t[:, :], in0=KHAT.t[:, :], scalar1=CF.t[:, K_IND64 + c:K_IND64 + c + 1], scalar2=None,
                                                          op0=ALU.mult), r=[KHAT, CF], w=[KHC[c]])
                ub = bank()
                for h in range(4):
                    op('pe', lambda e, h=h, c=c, ub=ub: e.matmul(ub.t[:, h * 128:(h + 1) * 128], lhsT=KHC[c].t[:, h * 128:(h + 1) * 128],
                                                         rhs=VB.t[:, h * 128:(h + 1) * 128], start=True, stop=True),
                       r=[KHC[c], VB], w=[ub])
                for h in range(4):
                    op('dve', lambda e, h=h, c=c, ub=ub: e.scalar_tensor_tensor(out=SF.t[:, h, :], in0=SF.t[:, h, :], scalar=DEC.t[:, h * 2 + c:h * 2 + c + 1],
                                                                      in1=ub.t[:, h * 128:(h + 1) * 128], op0=ALU.mult, op1=ALU.add),
                       r=[SF, DEC, ub], w=[SF])
                op('act', lambda e, s_out=s_out: e.copy(out=s_out.t[:, :, :], in_=SF.t[:, :, :]), r=[SF], w=[s_out])
            if CUT == 8:
                return
            if own:
                ob = bank()
                for h in range(4):
                    op('pe', lambda e, h=h: e.matmul(ob.t[:, h * 128:(h + 1) * 128], lhsT=VB.t[:, h * 128:(h + 1) * 128], rhs=AT.t[:, h, :],
                                                     start=True, stop=False), r=[VB, AT], w=[ob])
                    op('pe', lambda e, h=h: e.matmul(ob.t[:, h * 128:h * 128 + 64], lhsT=s_start.t[:, h, :], rhs=QTT.t[:, h, 0:64],
                                                     start=False, stop=False), r=[s_start, QTT], w=[ob])
                    op('pe', lambda e, h=h: e.matmul(ob.t[:, h * 128 + 64:h * 128 + 128], lhsT=s_mid.t[:, h, :], rhs=QTT.t[:, h, 64:128],
                                                     start=False, stop=True), r=[s_mid, QTT], w=[ob])
                obh = [(ob, h * 128) for h in range(4)]
        else:
            obs = [bank() for _ in range(4)]
            st['banks'] = [i for i in range(6) if all(PB[i] is not o_ for o_ in obs)]
            for h in range(4):
                op('pe', lambda e, h=h: e.matmul(obs[h].t[:, 0:128], lhsT=VB.t[:, h * 128:(h + 1) * 128], rhs=AT.t[:, h, :],
                                                 start=True, stop=False), r=[VB, AT], w=[obs[h]])
            for bq in range(NSEQ):
                sf = S0F[bq % 2]; sb_ = S0B[bq % 2]; vm = VM[bq % 2]; sn = SNEW[bq % 2]
                dma('sp', sf.t[:, :, :], st0[bq * 512:(bq + 1) * 512, :].rearrange("(h p) e -> p h e", p=128), r=[], w=[sf])
                op('act', lambda e, sf=sf, sb_=sb_: e.copy(out=sb_.t[:, :, :], in_=sf.t[:, :, :]), r=[sf], w=[sb_])
                for h in range(4):
                    op('pe', lambda e, h=h, bq=bq, sb_=sb_: e.matmul(obs[h].t[:, 4 * bq:4 * bq + 4], lhsT=sb_.t[:, h, :], rhs=QTT.t[:, h, 4 * bq:4 * bq + 4],
                                                                 start=False, stop=(bq == NSEQ - 1)), r=[sb_, QTT], w=[obs[h]])
                op('pool', lambda e, bq=bq, vm=vm: e.tensor_scalar(out=vm.t[:, :], in0=VB.t[:, :], scalar1=CF.t[:, K_IND4 + bq:K_IND4 + bq + 1], scalar2=None,
                                                               op0=ALU.mult), r=[VB, CF], w=[vm])
                ub = bank()
                for h in range(4):
                    op('pe', lambda e, h=h, vm=vm, ub=ub: e.matmul(ub.t[:, h * 128:(h + 1) * 128], lhsT=KHAT.t[:, h * 128:(h + 1) * 128],
                                                           rhs=vm.t[:, h * 128:(h + 1) * 128], start=True, stop=True), r=[KHAT, vm], w=[ub])
                for h in range(4):
                    op('dve', lambda e, h=h, bq=bq, sf=sf, sn=sn, ub=ub: e.scalar_tensor_tensor(
                        out=sn.t[:, h, :], in0=sf.t[:, h, :], scalar=DEC.t[:, h * NCK + bq:h * NCK + bq + 1],
                        in1=ub.t[:, h * 128:(h + 1) * 128], op0=ALU.mult, op1=ALU.add), r=[sf, DEC, ub], w=[sn])
                dma('sp', sts[bq * 512:(bq + 1) * 512, :].rearrange("(h p) e -> p h e", p=128), sn.t[:, :, :], r=[sn], w=[])
            obh = [(obs[h], 0) for h in range(4)]
        if not own:
            return
        if CUT == 9:
            return
        for h in range(4):
            ob_, c0 = obh[h]
            op('act', lambda e, h=h, ob_=ob_, c0=c0: e.activation(out=SQ.t[:, h * 128:(h + 1) * 128], in_=ob_.t[:, c0:c0 + 128], func=AF.Square),
               r=[ob_], w=[SQ])
        ssb = bank()
        op('pe', lambda e: e.matmul(ssb.t[:, :], lhsT=CB.t[:, K_ONES:K_ONES + 128], rhs=SQ.t[:, :], start=True, stop=True), r=[CB, SQ], w=[ssb])
        ro = tmp()
        op('act', lambda e: e.activation(out=ro.t[:, :], in_=ssb.t[:, :], func=AF.Sqrt, scale=1.0 / 128, bias=EPSC), r=[ssb], w=[ro])
        op('dve', lambda e: e.reciprocal(out=ro.t[:, :], in_=ro.t[:, :]), r=[ro], w=[ro])
        gb_ = bank()
        for h in range(4):
            proj_feat(A_HOG + h * 128, 128, gb_, h * 128)
        sil = tmp()
        op('act', lambda e: e.activation(out=sil.t[:, :], in_=gb_.t[:, :], func=AF.Silu), r=[gb_], w=[sil])
        o1 = tmp()
        for h in range(4):
            ob_, c0 = obh[h]
            op('dve', lambda e, h=h, ob_=ob_, c0=c0: e.tensor_tensor(out=o1.t[:, h * 128:(h + 1) * 128], in0=ob_.t[:, c0:c0 + 128],
                                                                 in1=ro.t[:, h * 128:(h + 1) * 128], op=ALU.mult), r=[ob_, ro], w=[o1])
        st['banks'] = list(range(6))
        op('dve', lambda e: e.scalar_tensor_tensor(out=OF.t[:, :, :], in0=o1.t[:, :].rearrange("p (h t) -> p h t", h=4), scalar=HG.t[:, 0:1],
                                                   in1=sil.t[:, :].rearrange("p (h t) -> p h t", h=4), op0=ALU.mult, op1=ALU.mult),
           r=[o1, HG, sil], w=[OF])
        if CUT == 10:
            return
        for half in range(2):
            yb = bank()
            for h in range(4):
                op('pe', lambda e, h=h, half=half, yb=yb: e.matmul(yb.t[:, :], lhsT=OF.t[:, h, :],
                                                           rhs=W.t[:, WA0 + h * 1024 + half * 512:WA0 + h * 1024 + half * 512 + 512],
                                                           start=(h == 0), stop=(h == 3)), r=[OF, W], w=[yb])
            gab = proj_tok(A_GA + half * 512, 512)
            sg = tmp()
            op('act', lambda e, gab=gab, sg=sg: e.activation(out=sg.t[:, :], in_=gab.t[:, :], func=AF.Sigmoid), r=[gab], w=[sg])
            ma = tmp()
            op('dve', lambda e, yb=yb, sg=sg, ma=ma: e.tensor_tensor(out=ma.t[:, :], in0=yb.t[:, :], in1=sg.t[:, :], op=ALU.mult), r=[yb, sg], w=[ma])
            dma('sp', ma_scr[oi * 128:(oi + 1) * 128, half * 512:(half + 1) * 512], ma.t[:, :], r=[ma], w=[], dw=D_MA)

    STOP = int(_ENV.get("STOP", "99"))
    for j in range(min(NT, STOP)):
        X = load_x(xp[j * 128:(j + 1) * 128, :])
        if j % 4 == 3:
            pass1_tile(X, 'own', j, j // 4)
        else:
            pass1_tile(X, 'light', j, None)
    dma('sp', stp.rearrange("(h p) e -> p h e", p=128), SF.t[:, :, :], r=[SF], w=[])
    if STOP > 10:
        X = load_x(xs)
        pass1_tile(X, 'samp', None, NOWN)

    PASSES = int(_ENV.get("PASSES", "3"))
    if PASSES >= 2:
        old = new_pass()
        TP[:] = [carve("q%d" % i, [128, 512], F32) for i in range(5)]
        MK = [carve("mk%d" % i, [128, 512], BF16) for i in range(2)]
        KTC = [carve("ktc%d" % i, [128, 4, 512], BF16) for i in range(2)]
        VC = [carve("vc%d" % i, [128, 4, 512], BF16) for i in range(2)]
        IKC = [carve("ikc%d" % i, [64, 512], BF16) for i in range(2)]
        EX = [carve("ex%d" % i, [128, 512], BF16) for i in range(2)]
        PM = [carve("pm%d" % i, [128, 512], BF16) for i in range(2)]
        PTT = [carve("ptt%d" % i, [128, 4, 128], BF16) for i in range(2)]
        AQT = carve("aqT", [128, 4, 128], BF16); IQT = carve("iqT", [64, 8, 128], BF16)
        IW = carve("iw", [128, 8], F32); IWS = carve("iws", [128, 8], F32); THR = carve("thr", [128, 16], F32)
        NCHR = max(LP // 512, NPG + 1)
        RS = carve("rs", [128, 4 * NCHR], F32)
        AO = carve("ao", [128, 512], BF16); AOT = carve("aoT", [128, 4, 128], BF16)
        MB = carve("mb", [128, 1024], BF16); MT = carve("mT", [128, 8, 128], BF16)
        NPT = NSEQ * NPG
        PTI = carve("pti", [128, NPT], I32); PTF = carve("ptf", [128, NPT], F32); PIDX = carve("pidx", [128, NPT], I32)
        IOTA = carve("iota", [128, 8], I32); IOTF = carve("iotf", [128, 8], F32)
        PGB = [carve("pgb%d" % i, [128, 512], BF16) for i in range(2)]
        IPB = carve("ipb", [128, 64], BF16)
        fence(old + cur['bufs'] + [W])
        woff = load_w(w2, B_END, [(w_b, 4), (w_o, 8)])
        WB0, WO0 = woff
        IDX_SCALE = 512.0 ** -0.5
        rr = dict(i=0)

        def ring(lst):
            rr['i'] += 1
            return lst[rr['i'] % len(lst)]

        dma('sp', PTI.t[:, :], ptab.partition_broadcast(128), r=[], w=[PTI])
        op('pool', lambda e: e.iota(IOTA.t[:, 0:1], pattern=[[0, 1]], base=0, channel_multiplier=1), r=[], w=[IOTA])
        op('dve', lambda e: e.tensor_copy(out=PTF.t[:, :], in_=PTI.t[:, :]), r=[PTI], w=[PTF])
        op('dve', lambda e: e.tensor_copy(out=IOTF.t[:, 0:1], in_=IOTA.t[:, 0:1]), r=[IOTA], w=[IOTF])
        op('dve', lambda e: e.tensor_scalar(out=PTF.t[:, :], in0=PTF.t[:, :], scalar1=128.0, scalar2=IOTF.t[:, 0:1], op0=ALU.mult, op1=ALU.add),
           r=[PTF, IOTF], w=[PTF])
        op('dve', lambda e: e.tensor_copy(out=PIDX.t[:, :], in_=PTF.t[:, :]), r=[PTF], w=[PIDX])

        class PromptProv:
            def ik(self, c, n):
                b = ring(IKC)
                dma('sp', b.t[0:64, 0:n], ikt_scr[:, c * 512:c * 512 + n], r=[], w=[b], dr=D_IKT)
                return b

            def kv(self, c, n):
                ktc = ring(KTC); vc = ring(VC)
                dma('sp', ktc.t[:, :, 0:n], kt_scr[:, :, c * 512:c * 512 + n].rearrange("h d l -> d h l"), r=[], w=[ktc], dr=D_KT)
                dma('sp', vc.t[:, 0:n // 128, :], v_scr[c * 512:c * 512 + n, :].rearrange("(t p) e -> p t e", p=128), r=[], w=[vc], dr=D_V)
                return ktc, vc

        class SampProv:
            def __init__(self, bq):
                self.bq = bq

            def ik(self, c, n):
                bq = self.bq
                b = ring(IKC)
                if c < NPG:
                    pg = tmp()
                    dma('pool', pg.t[:, 0:64], cik, r=[PIDX], w=[pg], indirect=PIDX.t[:, bq * NPG + c:bq * NPG + c + 1])
                    op('pool', lambda e, pg=pg: e.tensor_copy(out=IPB.t[:, :], in_=pg.t[:, 0:64]), r=[pg], w=[IPB])
                    tb = trbank()
                    op('pe', lambda e, tb=tb: e.transpose(out=tb.t[0:64, 0, :], in_=IPB.t[:, 0:64], identity=IDB), r=[IPB, CB], w=[tb])
                    op('act', lambda e, tb=tb, b=b: e.copy(out=b.t[0:64, 0:128], in_=tb.t[0:64, 0, :]), r=[tb], w=[b])
                else:
                    op('pool', lambda e, b=b: e.memset(b.t[0:64, 0:128], 0.0), r=[], w=[b])
                    dma('sp', b.t[0:64, 0:4], iks_scr[:, 4 * bq:4 * bq + 4], r=[], w=[b], dr=D_S)
                return b

            def kv(self, c, n):
                bq = self.bq
                ktc = ring(KTC); vc = ring(VC)
                if c < NPG:
                    col = PIDX.t[:, bq * NPG + c:bq * NPG + c + 1]
                    pg = tmp()
                    dma('pool', pg.t[:, :], ck, r=[PIDX], w=[pg], indirect=col)
                    pgb = ring(PGB)
                    op('pool', lambda e, pg=pg, pgb=pgb: e.tensor_copy(out=pgb.t[:, :], in_=pg.t[:, :]), r=[pg], w=[pgb])
                    tb = trbank()
                    for h in range(4):
                        op('pe', lambda e, h=h, tb=tb, pgb=pgb: e.transpose(out=tb.t[:, h, :], in_=pgb.t[:, h * 128:(h + 1) * 128], identity=IDB),
                           r=[pgb, CB], w=[tb])
                    op('act', lambda e, tb=tb, ktc=ktc: e.copy(out=ktc.t[:, :, 0:128], in_=tb.t[:, 0:4, :]), r=[tb], w=[ktc])
                    pg2 = tmp()
                    dma('pool', pg2.t[:, :], cv, r=[PIDX], w=[pg2], indirect=col)
                    op('pool', lambda e, pg2=pg2, vc=vc: e.tensor_copy(out=vc.t[:, 0, :], in_=pg2.t[:, :]), r=[pg2], w=[vc])
                else:
                    op('pool', lambda e, ktc=ktc: e.memset(ktc.t[:, :, 0:128], 0.0), r=[], w=[ktc])
                    dma('sp', ktc.t[:, :, 0:4], kts_scr[:, :, 4 * bq:4 * bq + 4].rearrange("h d t -> d h t"), r=[], w=[ktc], dr=D_S)
                    op('pool', lambda e, vc=vc: e.memset(vc.t[:, 0, :], 0.0), r=[], w=[vc])
                    dma('sp', vc.t[0:4, 0, :], vs_scr[4 * bq:4 * bq + 4, :], r=[], w=[vc], dr=D_S)
                return ktc, vc

        SCb = U1

        def attend(nq, qc0, iwb, L, ksel, prov, CH, prompt):
            SC = U1.t
            nch = (L + CH - 1) // CH
            IDQ = CB.t[0:nq, K_ID:K_ID + nq]
            for c in range(nch):
                n = min(CH, L - c * CH)
                ikc = prov.ik(c, n)
                for h in range(8):
                    b = bank()
                    op('pe', lambda e, b=b, h=h, ikc=ikc, n=n: e.matmul(b.t[0:nq, 0:n], lhsT=IQT.t[0:64, h, qc0:qc0 + nq], rhs=ikc.t[0:64, 0:n],
                                                                       start=True, stop=True), r=[IQT, ikc], w=[b])
                    rl = tmp()
                    op('act', lambda e, b=b, rl=rl, n=n: e.activation(out=rl.t[0:nq, 0:n], in_=b.t[0:nq, 0:n], func=AF.Relu), r=[b], w=[rl])
                    if h == 0:
                        op('dve', lambda e, rl=rl, c=c, n=n: e.tensor_scalar(out=SC[0:nq, c * CH:c * CH + n], in0=rl.t[0:nq, 0:n], scalar1=iwb.t[0:nq, 0:1],
                                                                          scalar2=None, op0=ALU.mult), r=[rl, iwb], w=[SCb])
                    else:
                        op('dve', lambda e, rl=rl, c=c, n=n, h=h: e.scalar_tensor_tensor(out=SC[0:nq, c * CH:c * CH + n], in0=rl.t[0:nq, 0:n],
                                                                                      scalar=iwb.t[0:nq, h:h + 1], in1=SC[0:nq, c * CH:c * CH + n],
                                                                                      op0=ALU.mult, op1=ALU.add), r=[rl, iwb, SCb], w=[SCb])
            T = THR.t
            op('dve', lambda e: e.tensor_reduce(out=T[0:nq, 0:1], in_=SC[0:nq, 0:L], axis=AX.X, op=ALU.max), r=[SCb], w=[THR])
            op('dve', lambda e: e.tensor_reduce(out=T[0:nq, 1:2], in_=SC[0:nq, 0:L], axis=AX.X, op=ALU.min), r=[SCb], w=[THR])
            if prompt:
                for t in range(3):
                    op('dve', lambda e, t=t: e.tensor_scalar(out=SC[0:nq, t * 128:(t + 1) * 128], in0=SC[0:nq, t * 128:(t + 1) * 128],
                                                            scalar1=PADB.t[0:nq, t:t + 1], scalar2=None, op0=ALU.add), r=[SCb, PADB], w=[SCb])
            op('dve', lambda e: e.tensor_tensor(out=SC[0:nq, L - 128:L], in0=SC[0:nq, L - 128:L], in1=CF.t[0:nq, K_CNEG:K_CNEG + 128], op=ALU.add),
               r=[SCb, CF], w=[SCb])
            op('dve', lambda e: e.tensor_tensor(out=T[0:nq, 2:3], in0=T[0:nq, 0:1], in1=T[0:nq, 1:2], op=ALU.subtract), r=[THR], w=[THR])
            op('dve', lambda e: e.tensor_copy(out=T[0:nq, 3:4], in_=T[0:nq, 1:2]), r=[THR], w=[THR])
            for it in range(1, NIT + 1):
                sc = 2.0 ** -it
                op('dve', lambda e, sc=sc: e.scalar_tensor_tensor(out=T[0:nq, 4:5], in0=T[0:nq, 2:3], scalar=sc, in1=T[0:nq, 3:4], op0=ALU.mult, op1=ALU.add),
                   r=[THR], w=[THR])
                op('dve', lambda e: e.tensor_scalar(out=JK.t[0:nq, 0:L], in0=SC[0:nq, 0:L], scalar1=T[0:nq, 4:5], scalar2=None, op0=ALU.is_ge, op1=ALU.add,
                                                    accum_out=T[0:nq, 5:6]), r=[SCb, THR], w=[JK, THR])
                op('dve', lambda e: e.tensor_scalar(out=T[0:nq, 6:7], in0=T[0:nq, 5:6], scalar1=float(ksel) - 0.5, scalar2=None, op0=ALU.is_ge),
                   r=[THR], w=[THR])
                op('dve', lambda e: e.tensor_tensor(out=T[0:nq, 7:8], in0=T[0:nq, 6:7], in1=T[0:nq, 2:3], op=ALU.mult), r=[THR], w=[THR])
                op('dve', lambda e, sc=sc: e.scalar_tensor_tensor(out=T[0:nq, 3:4], in0=T[0:nq, 7:8], scalar=sc, in1=T[0:nq, 3:4], op0=ALU.mult, op1=ALU.add),
                   r=[THR], w=[THR])
            st['banks'] = [4, 5]
            OA = PB[0:4]
            op('pool', lambda e: e.memset(RS.t[:, :], 0.0), r=[], w=[RS])
            for c in range(nch):
                n = min(CH, L - c * CH)
                nt = n // 128
                ktc, vc = prov.kv(c, n)
                mk = ring(MK)
                op('dve', lambda e, mk=mk, c=c, n=n: e.tensor_scalar(out=mk.t[0:nq, 0:n], in0=SC[0:nq, c * CH:c * CH + n], scalar1=T[0:nq, 3:4], scalar2=None,
                                                                  op0=ALU.is_ge), r=[SCb, THR], w=[mk])
                for h in range(4):
                    lg = bank()
                    op('pe', lambda e, lg=lg, h=h, ktc=ktc, n=n: e.matmul(lg.t[0:nq, 0:n], lhsT=AQT.t[:, h, qc0:qc0 + nq], rhs=ktc.t[:, h, 0:n],
                                                                         start=True, stop=True), r=[AQT, ktc], w=[lg])
                    ex = ring(EX); pm = ring(PM); pt = ring(PTT)
                    op('act', lambda e, lg=lg, ex=ex, n=n: e.activation(out=ex.t[0:nq, 0:n], in_=lg.t[0:nq, 0:n], func=AF.Exp), r=[lg], w=[ex])
                    op('dve', lambda e, ex=ex, pm=pm, mk=mk, n=n, c=c, h=h: e.scalar_tensor_tensor(
                        out=pm.t[0:nq, 0:n], in0=ex.t[0:nq, 0:n], scalar=1.0, in1=mk.t[0:nq, 0:n], op0=ALU.mult, op1=ALU.mult,
                        accum_out=RS.t[0:nq, h * NCHR + c:h * NCHR + c + 1]), r=[ex, mk], w=[pm, RS])
                    tb = trbank()
                    for t in range(nt):
                        op('pe', lambda e, tb=tb, pm=pm, t=t: e.transpose(out=tb.t[:, t, 0:nq], in_=pm.t[0:nq, t * 128:(t + 1) * 128], identity=IDQ),
                           r=[pm, CB], w=[tb])
                    op('act', lambda e, tb=tb, pt=pt, nt=nt: e.copy(out=pt.t[:, 0:nt, 0:nq], in_=tb.t[:, 0:nt, 0:nq]), r=[tb], w=[pt])
                    for t in range(nt):
                        op('pe', lambda e, pt=pt, vc=vc, t=t, h=h, c=c, nt=nt: e.matmul(
                            OA[h].t[0:nq, 0:128], lhsT=pt.t[:, t, 0:nq], rhs=vc.t[:, t, h * 128:(h + 1) * 128],
                            start=(c == 0 and t == 0), stop=(c == nch - 1 and t == nt - 1)), r=[pt, vc], w=[OA[h]])
            if _ENV.get("DBG") == "1" and not prompt and qc0 == 0:
                dma('sp', ks[0:4, 0:L], U1.t[0:4, 0:L], r=[U1], w=[])
                dma('sp', ks[4:8, 0:16], THR.t[0:4, 0:16], r=[THR], w=[])
                dma('sp', ks[8:12, 0:4 * nch], RS.t[0:4, 0:4 * nch], r=[RS], w=[])
            op('dve', lambda e: e.tensor_reduce(out=T[0:nq, 8:12], in_=RS.t[0:nq, 0:4 * NCHR].rearrange("p (h c) -> p h c", h=4), axis=AX.X, op=ALU.add),
               r=[RS], w=[THR])
            op('dve', lambda e: e.reciprocal(out=T[0:nq, 12:16], in_=T[0:nq, 8:12]), r=[THR], w=[THR])
            for h in range(4):
                op('act', lambda e, h=h: e.activation(out=AO.t[0:nq, h * 128:(h + 1) * 128], in_=OA[h].t[0:nq, 0:128], func=AF.Copy,
                                                      scale=T[0:nq, 12 + h:13 + h]), r=[OA[h], THR], w=[AO])
            tb = trbank()
            for h in range(4):
                op('pe', lambda e, tb=tb, h=h: e.transpose(out=tb.t[:, h, 0:nq], in_=AO.t[0:nq, h * 128:(h + 1) * 128], identity=IDQ), r=[AO, CB], w=[tb])
            op('act', lambda e, tb=tb: e.copy(out=AOT.t[:, :, qc0:qc0 + nq], in_=tb.t[:, 0:4, 0:nq]), r=[tb], w=[AOT])
            st['banks'] = list(range(6))

        def pass2_tile(X, oi, samp):
            to_hT(X, G1)
            ab = bank()
            for h in range(4):
                proj_feat(B_AQ + h * 128, 128, ab, h * 128)
            op('act', lambda e: e.activation(out=AQT.t[:, :, :], in_=ab.t[:, :].rearrange("p (h t) -> p h t", h=4), func=AF.Copy, scale=128.0 ** -0.5),
               r=[ab], w=[AQT])
            for g in range(2):
                ibk = bank()
                for hh in range(4):
                    proj_feat(B_IQ + (g * 4 + hh) * 64, 64, ibk, hh * 128)
                op('act', lambda e, g=g, ibk=ibk: e.copy(out=IQT.t[0:64, g * 4:(g + 1) * 4, :], in_=ibk.t[0:64, :].rearrange("p (h t) -> p h t", h=4)),
                   r=[ibk], w=[IQT])
            wb_ = proj_tok(B_IW, 8)
            op('dve', lambda e: e.tensor_scalar(out=IW.t[:, :], in0=wb_.t[:, 0:8], scalar1=IDX_SCALE, scalar2=None, op0=ALU.mult), r=[wb_], w=[IW])
            if not samp:
                attend(128, 0, IW, (4 * oi + 4) * 128, KSEL_P, PromptProv(), 512, True)
            else:
                op('pool', lambda e: e.memset(AOT.t[:, :, :], 0.0), r=[], w=[AOT])
                for bq in range(NSEQ):
                    dma('sp', IWS.t[0:4, :], IW.t[4 * bq:4 * bq + 4, :], r=[IW], w=[IWS])
                    attend(4, 4 * bq, IWS, LS, KSEL_S, SampProv(bq), 128, False)
            if _ENV.get("DBG") == "2" and samp:
                dma('pool', ks[:, :].rearrange("p (h t) -> p h t", h=4), AOT.t[:, :, :], r=[AOT], w=[])
            for half in range(2):
                yb = bank()
                for h in range(4):
                    op('pe', lambda e, h=h, half=half, yb=yb: e.matmul(yb.t[:, :], lhsT=AOT.t[:, h, :],
                                                                      rhs=W.t[:, WB0 + h * 1024 + half * 512:WB0 + h * 1024 + half * 512 + 512],
                                                                      start=(h == 0), stop=(h == 3)), r=[AOT, W], w=[yb])
                gbb = proj_tok(B_GB + half * 512, 512)
                sg = tmp(); m = tmp(); mat = tmp()
                op('act', lambda e, gbb=gbb, sg=sg: e.activation(out=sg.t[:, :], in_=gbb.t[:, :], func=AF.Sigmoid), r=[gbb], w=[sg])
                op('dve', lambda e, yb=yb, sg=sg, m=m: e.tensor_tensor(out=m.t[:, :], in0=yb.t[:, :], in1=sg.t[:, :], op=ALU.mult), r=[yb, sg], w=[m])
                dma('sp', mat.t[:, :], ma_scr[oi * 128:(oi + 1) * 128, half * 512:(half + 1) * 512], r=[], w=[mat], dr=D_MA)
                op('pool', lambda e, m=m, mat=mat, half=half: e.tensor_tensor(out=MB.t[:, half * 512:(half + 1) * 512], in0=m.t[:, :], in1=mat.t[:, :], op=ALU.add),
                   r=[m, mat], w=[MB])
            if _ENV.get("DBG") == "3" and samp:
                dma('pool', ks[:, :], MB.t[:, 0:512], r=[MB], w=[])
                dma('pool', vs[:, :], MB.t[:, 512:1024], r=[MB], w=[])
            tb = trbank()
            for k in range(8):
                op('pe', lambda e, k=k, tb=tb: e.transpose(out=tb.t[:, k, :], in_=MB.t[:, k * 128:(k + 1) * 128], identity=IDB), r=[MB, CB], w=[tb])
            op('act', lambda e, tb=tb: e.copy(out=MT.t[:, :, :], in_=tb.t[:, :, :]), r=[tb], w=[MT])
            for half in range(2):
                xb = bank()
                for k in range(8):
                    op('pe', lambda e, k=k, half=half, xb=xb: e.matmul(xb.t[:, :], lhsT=MT.t[:, k, :],
                                                                      rhs=W.t[:, WO0 + k * 1024 + half * 512:WO0 + k * 1024 + half * 512 + 512],
                                                                      start=(k == 0), stop=(k == 7)), r=[MT, W], w=[xb])
                x1 = tmp()
                op('dve', lambda e, xb=xb, x1=x1, half=half, X=X: e.tensor_tensor(out=x1.t[:, :], in0=xb.t[:, :], in1=X.t[:, half * 512:(half + 1) * 512], op=ALU.add),
                   r=[xb, X], w=[x1])
                dma('sp', x1_scr[oi * 128:(oi + 1) * 128, half * 512:(half + 1) * 512], x1.t[:, :], r=[x1], w=[], dw=D_X1)
                if PASSES == 2:
                    dst = ys if samp else yp[oi * 128:(oi + 1) * 128, :]
                    dma('sp', dst[:, half * 512:(half + 1) * 512], x1.t[:, :], r=[x1], w=[])

        for oi in range(NOWN):
            X = load_x(xp[(4 * oi + 3) * 128:(4 * oi + 4) * 128, :])
            pass2_tile(X, oi, False)
        X = load_x(xs)
        pass2_tile(X, NOWN, True)

    if PASSES >= 3:
        old = new_pass()
        TP[:] = [carve("r%d" % i, [128, 512], F32) for i in range(4)]
        X1T = [carve("x1t%d" % i, [128, 1024], F32) for i in range(4)]
        H2T = carve("h2T", [128, 8, 512], BF16)
        X2 = [carve("x2_%d" % i, [128, 1024], F32) for i in range(2)]
        YT = [carve("yt%d" % i, [128, 1024], F32) for i in range(2)]
        HID = U1.t[:, 0:8192].bitcast(BF16).rearrange("p (c t) -> p c t", c=32)
        fence(old + cur['bufs'] + [W, U1, G1, LB, OML, G2, GF])
        dma('sp', G2.t[:, :], n2.partition_broadcast(128), r=[], w=[G2])
        dma('sp', GF.t[:, :], nf.partition_broadcast(128), r=[], w=[GF])
        NTL = NOWN + 1
        GS = int(_ENV.get("GS", "4"))
        for g0 in range(0, NTL, GS):
            tiles = list(range(g0, min(NTL, g0 + GS)))
            ntok = len(tiles) * 128
            for ti, t in enumerate(tiles):
                Xt = X1T[ti]
                dma('sp', Xt.t[:, :], x1_scr[t * 128:(t + 1) * 128, :], r=[], w=[Xt], dr=D_X1)
                rstd = rmsnorm_rstd(Xt)
                op('dve', lambda e, Xt=Xt, rstd=rstd: e.scalar_tensor_tensor(out=H.t[:, :], in0=Xt.t[:, :], scalar=rstd, in1=G2.t[:, :], op0=ALU.mult, op1=ALU.mult),
                   r=[Xt, SS, G2], w=[H])
                tb = trbank()
                for k in range(8):
                    op('pe', lambda e, k=k, tb=tb: e.transpose(out=tb.t[:, k, :], in_=H.t[:, k * 128:(k + 1) * 128], identity=IDB), r=[H, CB], w=[tb])
                op('act', lambda e, tb=tb, ti=ti: e.copy(out=H2T.t[:, :, ti * 128:(ti + 1) * 128], in_=tb.t[:, :, :]), r=[tb], w=[H2T])
            for k in range(8):
                for c0 in range(0, 4096, 2048):
                    dma('pool', W.t[:, k * 4096 + c0:k * 4096 + c0 + 2048], w_up[k * 128:(k + 1) * 128, c0:c0 + 2048], r=[], w=[W])
            for c in range(32):
                hb = bank()
                for k in range(8):
                    op('pe', lambda e, k=k, c=c, hb=hb, ntok=ntok: e.matmul(hb.t[:, 0:ntok], lhsT=W.t[:, k * 4096 + c * 128:k * 4096 + (c + 1) * 128], rhs=H2T.t[:, k, 0:ntok],
                                                              start=(k == 0), stop=(k == 7)), r=[W, H2T], w=[hb])
                rl = tmp()
                op('act', lambda e, hb=hb, rl=rl, ntok=ntok: e.activation(out=rl.t[:, 0:ntok], in_=hb.t[:, 0:ntok], func=AF.Relu), r=[hb], w=[rl])
                op('pool', lambda e, rl=rl, c=c, ntok=ntok: e.tensor_tensor(out=HID[:, c, 0:ntok], in0=rl.t[:, 0:ntok], in1=rl.t[:, 0:ntok], op=ALU.mult), r=[rl], w=[U1])
                if _ENV.get("DBG") == "6" and c == 0 and g0 == 0:
                    dma('sp', ks[:, 0:ntok], rl.t[:, 0:ntok], r=[rl], w=[])
                    dma('pool', vs[:, 0:ntok], HID[:, 0, 0:ntok], r=[U1], w=[])
                    dma('pool', iks[:, 0:64], H2T.t[:, 0, 192:256], r=[H2T], w=[])
            for c in range(32):
                dma('pool', W.t[:, c * 1024:(c + 1) * 1024], w_dn[c * 128:(c + 1) * 128, :], r=[], w=[W])
            for ti, t in enumerate(tiles):
                Xt = X1T[ti]
                x2 = X2[ti % 2]; yt = YT[ti % 2]
                for half in range(2):
                    yb = bank()
                    for c in range(32):
                        op('pe', lambda e, c=c, half=half, yb=yb, ti=ti: e.matmul(yb.t[:, :], lhsT=HID[:, c, ti * 128:(ti + 1) * 128],
                                                                             rhs=W.t[:, c * 1024 + half * 512:c * 1024 + half * 512 + 512],
                                                                             start=(c == 0), stop=(c == 31)), r=[U1, W], w=[yb])
                    op('dve', lambda e, yb=yb, x2=x2, Xt=Xt, half=half: e.tensor_tensor(out=x2.t[:, half * 512:(half + 1) * 512], in0=yb.t[:, :],
                                                                                   in1=Xt.t[:, half * 512:(half + 1) * 512], op=ALU.add), r=[yb, Xt], w=[x2])
                rstd = rmsnorm_rstd(x2)
                op('dve', lambda e, x2=x2, yt=yt, rstd=rstd: e.scalar_tensor_tensor(out=yt.t[:, :], in0=x2.t[:, :], scalar=rstd, in1=GF.t[:, :], op0=ALU.mult, op1=ALU.mult),
                   r=[x2, SS, GF], w=[yt])
                DBGV = _ENV.get("DBG")
                src = {"4": Xt, "5": x2}.get(DBGV, yt)
                dma('sp', ys if t == NOWN else yp[t * 128:(t + 1) * 128, :], src.t[:, :], r=[src], w=[])

    P.finish()
    es.close()
    return nc


FULL_CFG = dict(NT=64, NSEQ=16, NPG=16, NPHYS=2560, KSEL_P=256, KSEL_S=256)
_NC_CACHE = {}


def _prep_core(c, cfg, inp):
    NT, NSEQ, NPG, NPHYS = cfg['NT'], cfg['NSEQ'], cfg['NPG'], cfg['NPHYS']
    s, r = c // 4, c % 4
    pad = 3 - r
    f32 = np.float32
    xp = np.zeros((NT * 128, 1024), f32)
    xp[pad * 128:] = inp['x_prompt'][s, :(NT - pad) * 128]
    padb = np.zeros((128, 4), f32)
    padb[:, :pad] = -BIG
    xs = np.zeros((128, 1024), f32)
    xs[:4 * NSEQ] = inp['x_sample'][c * NSEQ:(c + 1) * NSEQ].reshape(4 * NSEQ, 1024)
    wi = inp['w_in'][0]
    col = lambda name, n: wi[:, R_OFF[name]:R_OFF[name] + n]
    w1 = np.concatenate([col('hf', 512), col('hi', 512), col('av', 512), col('ak', 512), col('ik', 64),
                         col('hq', 512), col('hog', 512), col('ga', 1024)], axis=1)
    w2 = np.concatenate([col('aq', 512), col('iq', 512), col('iw', 8), col('gb', 1024)], axis=1)
    d = dict(
        xp=xp, xs=xs, padb=padb,
        st0=inp['state_hgrn'][0, c * NSEQ:(c + 1) * NSEQ].reshape(NSEQ * 512, 128),
        ptab=inp['page_table'][c * NSEQ:(c + 1) * NSEQ].reshape(1, NSEQ * NPG).astype(np.int32),
        ck=inp['cache_k'][0].reshape(NPHYS * 128, 512), cv=inp['cache_v'][0].reshape(NPHYS * 128, 512),
        cik=inp['cache_idx_k'][0].reshape(NPHYS * 128, 64),
        cst=make_consts(), w1=w1, w2=w2, w_a=inp['w_a'][0], w_b=inp['w_b'][0], w_o=inp['w_o'][0],
        w_up=inp['w_up'][0], w_dn=inp['w_down'][0], lbl=inp['lb_logits'], hgn=inp['hg_norm'][0].reshape(128, 1),
        n1=inp['norm1'][0].reshape(1, 1024), n2=inp['norm2'][0].reshape(1, 1024), nf=inp['norm_f'].reshape(1, 1024))
    return {k: np.ascontiguousarray(v) for k, v in d.items()}


def run_cfg(cfg, inp, names=None):
    key = tuple(sorted(cfg.items()))
    if key not in _NC_CACHE:
        _NC_CACHE[key] = build(cfg)
    nc = _NC_CACHE[key]
    cores = cfg.get('cores', tuple(range(8)))
    in_maps = [_prep_core(c, cfg, inp) for c in cores]
    res = run_bass_kernel_spmd(nc, in_maps, core_ids=list(range(len(cores))))
    out = [None] * 8
    for i, c in enumerate(cores):
        out[c] = res.results[i]
    return out


def assemble(cfg, res):
    NT, NSEQ = cfg['NT'], cfg['NSEQ']
    NOWN = NT // 4
    SEQ = NT * 128
    f32 = np.float32
    y_p = np.zeros((2, SEQ, 1024), f32); k_p = np.zeros((1, 2, SEQ, 4, 128), f32); v_p = np.zeros((1, 2, SEQ, 4, 128), f32)
    i_p = np.zeros((1, 2, SEQ, 64), f32); s_p = np.zeros((1, 2, 4, 128, 128), f32)
    nb = 8 * NSEQ
    y_s = np.zeros((nb, 4, 1024), f32); k_s = np.zeros((1, nb, 4, 4, 128), f32); v_s = np.zeros((1, nb, 4, 4, 128), f32)
    i_s = np.zeros((1, nb, 4, 64), f32); s_s = np.zeros((1, nb, 4, 128, 128), f32)
    for c in range(8):
        s, r = c // 4, c % 4
        o = res[c]
        if o is None:
            continue
        for oi in range(NOWN):
            j = 4 * oi + r
            sl = slice(j * 128, (j + 1) * 128)
            so = slice(oi * 128, (oi + 1) * 128)
            if 'yp' in o:
                y_p[s, sl] = o['yp'][so]
            k_p[0, s, sl] = o['kp'][so].reshape(128, 4, 128)
            v_p[0, s, sl] = o['vp'][so].reshape(128, 4, 128)
            i_p[0, s, sl] = o['ikp'][so]
        if r == 3:
            s_p[0, s] = o['stp'].reshape(4, 128, 128)
        bs = slice(c * NSEQ, (c + 1) * NSEQ)
        if 'ys' in o:
            y_s[bs] = o['ys'][:4 * NSEQ].reshape(NSEQ, 4, 1024)
        k_s[0, bs] = o['ks'][:4 * NSEQ].reshape(NSEQ, 4, 4, 128)
        v_s[0, bs] = o['vs'][:4 * NSEQ].reshape(NSEQ, 4, 4, 128)
        i_s[0, bs] = o['iks'][:4 * NSEQ].reshape(NSEQ, 4, 64)
        s_s[0, bs] = o['sts'].reshape(NSEQ, 4, 128, 128)
    return (y_p, y_s, k_p, v_p, i_p, s_p, k_s, v_s, i_s, s_s)


def kernel(**inputs):
    inp = {k: np.asarray(v) for k, v in inputs.items()}
    res = run_cfg(FULL_CFG, inp)
    return assemble(FULL_CFG, res)
```

```python
import os
import numpy as np
from contextlib import ExitStack
import concourse.bass as bass
import concourse.mybir as mybir
from concourse.bass_utils import run_bass_kernel_spmd

F32 = mybir.dt.float32
BF16 = mybir.dt.bfloat16
I32 = mybir.dt.int32
AF = mybir.ActivationFunctionType
ALU = mybir.AluOpType
AX = mybir.AxisListType

ENGS = ('pe', 'act', 'dve', 'pool', 'sp')
_ENV = os.environ if os.environ.get('MK_DEBUG') else {}


class Buf:
    def __init__(self, name, t):
        self.name = name
        self.t = t
        self.w = None
        self.r = []
        self.dcnt = 0
        self.stores = []


class Prog:
    def __init__(self, nc, es):
        self.nc = nc
        self.es = es
        self.ops = {e: [] for e in ENGS}
        self.cnt = {e: 0 for e in ENGS}
        self.seen = {e: {} for e in ENGS}
        self.dsems = {}
        self.nbuf = 0
        self.cbuf = None
        self.cvals = {}

    def sb(self, name, shape, dtype):
        t = self.es.enter_context(self.nc.sbuf_tensor(name, list(shape), dtype))
        return Buf(name, t)

    def ps(self, name, shape, dtype):
        t = self.es.enter_context(self.nc.psum_tensor(name, list(shape), dtype))
        return Buf(name, t)

    def view(self, name, t):
        return Buf(name, t)

    def const_col(self, val):
        if self.cbuf is None:
            self.cbuf = self.sb("constcols", [128, 32], F32)
        if val not in self.cvals:
            i = len(self.cvals)
            assert i < 32
            self.cvals[val] = i
            self.op('pool', lambda e, i=i, val=val: e.memset(self.cbuf.t[:, i:i + 1], float(val)), r=[], w=[self.cbuf])
        i = self.cvals[val]
        return self.cbuf.t[:, i:i + 1]

    def _deps(self, eng, r, w):
        deps = {}
        def add(tok):
            if tok is None:
                return
            k, v = tok
            if k == eng and eng == 'pe':
                return
            if deps.get(k, 0) < v:
                deps[k] = v
        for b in r:
            add(b.w)
        for b in w:
            add(b.w)
            for tok in b.r:
                add(tok)
        waits = []
        seen = self.seen[eng]
        for k, v in deps.items():
            if seen.get(k, 0) >= v:
                continue
            seen[k] = v
            waits.append((k, v))
        return waits

    def _commit(self, tok, r, w):
        ws = set(id(b) for b in w)
        for b in w:
            b.w = tok
            b.r = []
        for b in r:
            if id(b) not in ws:
                b.r.append(tok)

    def op(self, eng, fn, r, w):
        if self.cbuf is not None and self.cbuf not in r and self.cbuf not in w:
            r = list(r) + [self.cbuf]
        waits = self._deps(eng, r, w)
        self.cnt[eng] += 1
        tok = (eng, self.cnt[eng])
        self.ops[eng].append((waits, fn, (eng, 1)))
        self._commit(tok, r, w)

    def dma(self, q, out_ap, in_ap, r, w, indirect=None, sem=None, dw=None, dr=None):
        sb = sem if sem is not None else (w[0] if w else r[0])
        waits = self._deps(q, r, w)
        if dr is not None:
            seen = self.seen[q]
            for (k, v) in dr.stores:
                if seen.get(k, 0) < v:
                    seen[k] = v
                    waits = [x for x in waits if x[0] != k] + [(k, v)]
        sb.dcnt += 1
        key = ('d', sb.name)
        self.dsems[sb.name] = sb
        tok = (key, 16 * sb.dcnt)
        if indirect is None:
            fn = lambda e: e.dma_start(out=out_ap, in_=in_ap)
        else:
            fn = lambda e: e.indirect_dma_start(out=out_ap, out_offset=None, in_=in_ap,
                                                in_offset=bass.IndirectOffsetOnAxis(ap=indirect, axis=0))
        self.ops[q].append((waits, fn, (key, 16)))
        self._commit(tok, r, w)
        if dw is not None:
            dw.stores.append(tok)

    def finish(self):
        nc = self.nc
        sem = {}
        for e in ENGS:
            sem[e] = self.es.enter_context(nc.semaphore("s_" + e))
        for name in self.dsems:
            sem[('d', name)] = self.es.enter_context(nc.semaphore("d_" + name))
        final = [(('d', n), 16 * b.dcnt) for n, b in self.dsems.items()]
        final += [(e, self.cnt[e]) for e in ENGS if e != 'sp' and self.cnt[e] > 0]

        def replay(ename, e):
            for waits, fn, inc in self.ops[ename]:
                for k, v in waits:
                    e.wait_ge(sem[k], v)
                ins = fn(e)
                ins.then_inc(sem[inc[0]], inc[1])
            if ename == 'sp':
                for k, v in final:
                    e.wait_ge(sem[k], v)

        with nc.Block() as block:
            @block.tensor
            def _(e):
                replay('pe', e)

            @block.scalar
            def _(e):
                replay('act', e)

            @block.vector
            def _(e):
                replay('dve', e)

            @block.gpsimd
            def _(e):
                replay('pool', e)

            @block.sync
            def _(e):
                replay('sp', e)


C_HF, C_HI, C_AV, C_AK, C_IK, C_HQ, C_HOG, C_AQ, C_IQ, C_IW, C_GA, C_GB, C_END = (
    0, 512, 1024, 1536, 2048, 2112, 2624, 3136, 3648, 4160, 4168, 5192, 6216)
R_OFF = dict(hq=0, hf=512, hi=1024, hog=1536, aq=2048, ak=2560, av=3072, iq=3584, ik=4096, iw=4160,
             ga=4168, gb=5192)
BIG = 1.0e30
EPS = 1e-6
K_ID, K_U64, K_L64, K_U4, K_L4, K_CNEG, K_IND64, K_IND4, K_ONES, K_END = (
    0, 128, 256, 384, 512, 640, 768, 770, 802, 930)


def make_consts():
    c = np.zeros((128, 1024), np.float32)
    p = np.arange(128)[:, None]
    j = np.arange(128)[None, :]
    c[:, K_ID:K_ID + 128] = (p == j)
    for (ku, kl, C) in ((K_U64, K_L64, 64), (K_U4, K_L4, 4)):
        same = (p // C) == (j // C)
        c[:, ku:ku + 128] = same & (p <= j)
        c[:, kl:kl + 128] = same & (p > j)
    c[:, K_CNEG:K_CNEG + 128] = np.where(j <= p, 0.0, -BIG)
    c[:, K_IND64:K_IND64 + 2] = (p // 64) == np.arange(2)[None, :]
    c[:, K_IND4:K_IND4 + 32] = (p // 4) == np.arange(32)[None, :]
    c[:, K_ONES:K_ONES + 128] = 1.0
    return c


A_HF, A_HI, A_AV, A_AK, A_IK, A_HQ, A_HOG, A_GA, A_END = 0, 512, 1024, 1536, 2048, 2112, 2624, 3136, 4160
B_AQ, B_IQ, B_IW, B_GB, B_END = 0, 512, 1024, 1032, 2056
ARENA = 8 * A_END + 4 * 1024


def build(cfg):
    NT, NSEQ, NPG, NPHYS = cfg['NT'], cfg['NSEQ'], cfg['NPG'], cfg['NPHYS']
    KSEL_P, KSEL_S = cfg['KSEL_P'], cfg['KSEL_S']
    NIT = cfg.get('NIT', 15)
    NOWN = NT // 4
    LP = NT * 128
    LS = (NPG + 1) * 128
    LMAX = max(LP, LS)
    NCH = LMAX // 512 + 1
    nc = bass.Bass("TRN2", target_bir_lowering=False)

    def din(name, shape, dt=F32):
        return nc.dram_tensor(name, list(shape), dt, kind="ExternalInput").ap()

    def dout(name, shape, dt=F32):
        return nc.dram_tensor(name, list(shape), dt, kind="ExternalOutput").ap()

    def dscr(name, shape, dt):
        return nc.dram_tensor(name, list(shape), dt, kind="Internal").ap()

    xp = din("xp", [LP, 1024]); xs = din("xs", [128, 1024]); padb = din("padb", [128, 4])
    st0 = din("st0", [NSEQ * 4 * 128, 128]); ptab = din("ptab", [1, NSEQ * NPG], I32)
    ck = din("ck", [NPHYS * 128, 512]); cv = din("cv", [NPHYS * 128, 512]); cik = din("cik", [NPHYS * 128, 64])
    cst = din("cst", [128, 1024]); w1 = din("w1", [1024, A_END]); w2 = din("w2", [1024, B_END])
    w_a = din("w_a", [512, 1024]); w_b = din("w_b", [512, 1024]); w_o = din("w_o", [1024, 1024])
    w_up = din("w_up", [1024, 4096]); w_dn = din("w_dn", [4096, 1024])
    lbl = din("lbl", [2, 512]); hgn = din("hgn", [128, 1]); n1 = din("n1", [1, 1024]); n2 = din("n2", [1, 1024])
    nf = din("nf", [1, 1024])
    yp = dout("yp", [NOWN * 128, 1024]); kp = dout("kp", [NOWN * 128, 512]); vp = dout("vp", [NOWN * 128, 512])
    ikp = dout("ikp", [NOWN * 128, 64]); stp = dout("stp", [4 * 128, 128])
    ys = dout("ys", [128, 1024]); ks = dout("ks", [128, 512]); vs = dout("vs", [128, 512])
    iks = dout("iks", [128, 64]); sts = dout("sts", [NSEQ * 4 * 128, 128])
    kt_scr = dscr("kt_scr", [4, 128, LP], BF16)
    v_scr = dscr("v_scr", [LP, 512], BF16)
    ma_scr = dscr("ma_scr", [(NOWN + 1) * 128, 1024], F32)
    x1_scr = dscr("x1_scr", [(NOWN + 1) * 128, 1024], F32)

    es = ExitStack()
    P = Prog(nc, es)
    op, dma = P.op, P.dma

    W = P.sb("arena", [128, ARENA], BF16)
    U1 = P.sb("u1", [128, 8192], F32)
    JK = P.sb("junk8", [128, 8192], mybir.dt.uint8)
    CF = P.sb("cf", [128, 1024], F32); CB = P.sb("cb", [128, 1024], BF16)
    GG = es.enter_context(nc.sbuf_tensor("gg", [128, 2048], F32))
    G1 = Buf("g1", GG[:, 0:1024]); LB = Buf("lb", GG[:, 1024:1536]); OML = Buf("oml", GG[:, 1536:2048])
    G2 = Buf("g2", GG[:, 0:1024]); GF = Buf("gf", GG[:, 1024:2048])
    HG = P.sb("hg", [128, 1], F32)
    PADB = P.sb("padbs", [128, 4], F32); SS = P.sb("ss", [128, 8], F32)
    XT = [P.sb("xt%d" % i, [128, 1024], F32) for i in range(2)]
    H = P.sb("h", [128, 1024], BF16); HT = P.sb("hT", [128, 8, 128], BF16)
    NSCR = 16 * 1024
    SCR = es.enter_context(nc.sbuf_tensor("scr", [128, NSCR], F32))
    cur = dict(off=0, bufs=[])

    def carve(name, shape, dt):
        n = int(np.prod(shape[1:]))
        nf32 = (n + 1) // 2 if dt == BF16 else n
        nf32 = (nf32 + 7) // 8 * 8
        o = cur['off']
        assert o + nf32 <= NSCR, (name, o, nf32)
        cur['off'] = o + nf32
        ap = SCR[0:shape[0], o:o + nf32]
        if dt == BF16:
            ap = ap.bitcast(BF16)[:, 0:n]
        else:
            ap = ap[:, 0:n]
        if dt == I32:
            ap = SCR[0:shape[0], o:o + nf32].bitcast(I32)[:, 0:n]
        if len(shape) == 3:
            ap = ap.rearrange("p (a b) -> p a b", a=shape[1])
        b = Buf(name, ap)
        cur['bufs'].append(b)
        return b

    def new_pass():
        old = cur['bufs']
        cur['off'] = 0
        cur['bufs'] = []
        return old

    def fence(bufs):
        if bufs:
            op('pool', lambda e: e.memset(SS.t[:, 7:8], 0.0), r=[], w=list(bufs) + [SS])

    PB = [P.ps("pb%d" % i, [128, 512], F32) for i in range(6)]
    PTR = [P.ps("ptr%d" % i, [128, 8, 128], BF16) for i in range(2)]
    st = dict(bank=0, banks=list(range(6)), tr=0, xi=0)

    def bank():
        b = PB[st['banks'][st['bank'] % len(st['banks'])]]
        st['bank'] += 1
        return b

    def trbank():
        b = PTR[st['tr'] % 2]
        st['tr'] += 1
        return b

    IDB = CB.t[:, K_ID:K_ID + 128]

    dma('sp', CF.t[:, :], cst, r=[], w=[CF])
    op('dve', lambda e: e.tensor_copy(out=CB.t[:, :], in_=CF.t[:, :]), r=[CF], w=[CB])
    dma('sp', G1.t[:, :], n1.partition_broadcast(128), r=[], w=[G1])
    dma('sp', LB.t[:, :], lbl[0:1, :].partition_broadcast(128), r=[], w=[LB])
    dma('sp', OML.t[:, :], lbl[1:2, :].partition_broadcast(128), r=[], w=[OML])
    dma('sp', HG.t[:, :], hgn, r=[], w=[HG])
    dma('sp', PADB.t[:, :], padb, r=[], w=[PADB])
    op('dve', lambda e: e.tensor_tensor(out=LB.t[:, :], in0=LB.t[:, :], in1=OML.t[:, :], op=ALU.subtract), r=[LB, OML], w=[LB])
    op('act', lambda e: e.activation(out=LB.t[:, :], in_=LB.t[:, :], func=AF.Sigmoid), r=[LB], w=[LB])
    op('dve', lambda e: e.tensor_scalar(out=OML.t[:, :], in0=LB.t[:, :], scalar1=-1.0, scalar2=1.0, op0=ALU.mult, op1=ALU.add), r=[LB], w=[OML])

    EPSC = P.const_col(EPS)
    WCOLS = dict(n=A_END)

    def Wc(k, off, n):
        return W.t[:, k * WCOLS['n'] + off:k * WCOLS['n'] + off + n]

    def load_w(src, ncols, extra):
        WCOLS['n'] = ncols
        for k in range(8):
            for c0 in range(0, ncols, 2048):
                c1 = min(ncols, c0 + 2048)
                dma('pool', W.t[:, k * ncols + c0:k * ncols + c1], src[k * 128:(k + 1) * 128, c0:c1], r=[], w=[W])
        o = 8 * ncols
        outs = []
        for (ap, nch) in extra:
            for k in range(nch):
                dma('pool', W.t[:, o + k * 1024:o + (k + 1) * 1024], ap[k * 128:(k + 1) * 128, :], r=[], w=[W])
            outs.append(o)
            o += nch * 1024
        assert o <= ARENA
        return outs

    def proj_tok(off, n):
        b = bank()
        for k in range(8):
            op('pe', lambda e, k=k, wap=Wc(k, off, n): e.matmul(b.t[:, 0:n], lhsT=HT.t[:, k, :], rhs=wap, start=(k == 0), stop=(k == 7)),
               r=[HT, W], w=[b])
        return b

    def proj_feat(off, n, b, c0):
        for k in range(8):
            op('pe', lambda e, k=k, wap=Wc(k, off, n): e.matmul(b.t[0:n, c0:c0 + 128], lhsT=wap, rhs=HT.t[:, k, :], start=(k == 0), stop=(k == 7)),
               r=[HT, W], w=[b])

    def rmsnorm_rstd(X, D=1024.0):
        op('dve', lambda e: e.scalar_tensor_tensor(out=JK.t[:, 0:1024], in0=X.t[:, :], scalar=1.0, in1=X.t[:, :], op0=ALU.mult, op1=ALU.mult,
                                                   accum_out=SS.t[:, 0:1]), r=[X], w=[JK, SS])
        op('dve', lambda e: e.tensor_copy(out=SS.t[:, 1:2], in_=SS.t[:, 0:1]), r=[SS], w=[SS])
        op('act', lambda e: e.activation(out=SS.t[:, 2:3], in_=SS.t[:, 1:2], func=AF.Sqrt, scale=1.0 / D,
                                         bias=EPSC), r=[SS], w=[SS])
        op('dve', lambda e: e.reciprocal(out=SS.t[:, 3:4], in_=SS.t[:, 2:3]), r=[SS], w=[SS])
        return SS.t[:, 3:4]

    def to_hT(X, G):
        rstd = rmsnorm_rstd(X)
        op('dve', lambda e: e.scalar_tensor_tensor(out=H.t[:, :], in0=X.t[:, :], scalar=rstd, in1=G.t[:, :], op0=ALU.mult, op1=ALU.mult),
           r=[X, SS, G], w=[H])
        tb = trbank()
        for k in range(8):
            op('pe', lambda e, k=k: e.transpose(out=tb.t[:, k, :], in_=H.t[:, k * 128:(k + 1) * 128], identity=IDB), r=[H, CB], w=[tb])
        op('act', lambda e: e.copy(out=HT.t[:, :, :], in_=tb.t[:, :, :]), r=[tb], w=[HT])

    def load_x(src_ap):
        X = XT[st['xi'] % 2]
        st['xi'] += 1
        dma('sp', X.t[:, :], src_ap, r=[], w=[X])
        return X

    D_V, D_KT, D_IKT, D_MA, D_X1, D_S = (Buf(n, None) for n in ("D_V", "D_KT", "D_IKT", "D_MA", "D_X1", "D_S"))
    ikt_scr = dscr("ikt_scr", [64, LP], BF16)
    kts_scr = dscr("kts_scr", [4, 128, 128], BF16)
    vs_scr = dscr("vs_scr", [128, 512], BF16)
    iks_scr = dscr("iks_scr", [64, 128], BF16)

    woff = load_w(w1, A_END, [(w_a, 4)])
    WA0 = woff[0]
    TP = [carve("tp%d" % i, [128, 512], F32) for i in range(6)]
    tpi = dict(i=0)

    def tmp():
        b = TP[tpi['i'] % len(TP)]
        tpi['i'] += 1
        return b

    LOGF = carve("logf", [128, 512], F32); KK = carve("kk", [128, 512], F32)
    VB = carve("vb", [128, 512], BF16); VA = carve("va", [128, 512], BF16)
    KHAT = carve("khat", [128, 512], BF16); QTL = carve("qtl", [128, 512], BF16); KTL = carve("ktl", [128, 512], BF16)
    QTT = carve("qtT", [128, 4, 128], BF16); KTT = carve("ktT", [128, 4, 128], BF16)
    AT = carve("at", [128, 4, 128], BF16); DEC = carve("dec", [128, 64], F32)
    SF = carve("sf", [128, 4, 128], F32); SBF = [carve("sbf%d" % i, [128, 4, 128], BF16) for i in range(3)]
    SQ = carve("sq", [128, 512], BF16); OF = carve("of", [128, 4, 128], BF16)
    KHC = [carve("khc%d" % i, [128, 512], BF16) for i in range(2)]
    KTILE = carve("ktile", [128, 4, 128], BF16); IKTILE = carve("iktile", [64, 128], BF16)
    IKF = carve("ikf", [128, 64], F32)
    S0F = [carve("s0f%d" % i, [128, 4, 128], F32) for i in range(2)]
    S0B = [carve("s0b%d" % i, [128, 4, 128], BF16) for i in range(2)]
    VM = [carve("vm%d" % i, [128, 512], BF16) for i in range(2)]
    SNEW = [carve("snew%d" % i, [128, 4, 128], F32) for i in range(2)]
    for i in range(3):
        op('pool', lambda e, i=i: e.memset(SBF[i].t[:, :, :], 0.0), r=[], w=[SBF[i]])
    op('pool', lambda e: e.memset(SF.t[:, :, :], 0.0), r=[], w=[SF])
    sidx = dict(i=0)

    CUT = int(_ENV.get("CUT", "0"))

    def pass1_tile(X, kind, j, oi):
        own = kind != 'light'
        samp = kind == 'samp'
        KU, KL, KIND, NCK = (K_U4, K_L4, K_IND4, NSEQ) if samp else (K_U64, K_L64, K_IND64, 2)
        to_hT(X, G1)
        b = proj_tok(A_HF, 512)
        sig = tmp()
        op('act', lambda e: e.activation(out=sig.t[:, :], in_=b.t[:, :], func=AF.Sigmoid), r=[b], w=[sig])
        f = tmp()
        op('pool', lambda e: e.tensor_tensor(out=f.t[:, :], in0=sig.t[:, :], in1=OML.t[:, :], op=ALU.mult), r=[sig, OML], w=[f])
        op('pool', lambda e: e.tensor_tensor(out=f.t[:, :], in0=f.t[:, :], in1=LB.t[:, :], op=ALU.add), r=[f, LB], w=[f])
        op('act', lambda e: e.activation(out=LOGF.t[:, :], in_=f.t[:, :], func=AF.Ln), r=[f], w=[LOGF])
        op('dve', lambda e: e.tensor_scalar(out=KK.t[:, :], in0=f.t[:, :], scalar1=-1.0, scalar2=1.0, op0=ALU.mult, op1=ALU.add), r=[f], w=[KK])
        if CUT == 1:
            return
        b2 = proj_tok(A_HI, 512)
        op('act', lambda e: e.copy(out=VB.t[:, :], in_=b2.t[:, :]), r=[b2], w=[VB])
        b3 = proj_tok(A_AV, 512)
        op('act', lambda e: e.copy(out=VA.t[:, :], in_=b3.t[:, :]), r=[b3], w=[VA])
        if samp:
            dma('sp', vs_scr, VA.t[:, :], r=[VA], w=[], dw=D_S)
        else:
            dma('sp', v_scr[j * 128:(j + 1) * 128, :], VA.t[:, :], r=[VA], w=[], dw=D_V)
        if own:
            t = tmp()
            op('dve', lambda e, t=t: e.tensor_copy(out=t.t[:, :], in_=b3.t[:, :]), r=[b3], w=[t])
            dma('sp', vs if samp else vp[oi * 128:(oi + 1) * 128, :], t.t[:, :], r=[t], w=[])
        if CUT == 2:
            return
        kb = bank()
        for h in range(4):
            proj_feat(A_AK + h * 128, 128, kb, h * 128)
        op('act', lambda e: e.copy(out=KTILE.t[:, :, :], in_=kb.t[:, :].rearrange("p (h t) -> p h t", h=4)), r=[kb], w=[KTILE])
        if samp:
            dma('sp', kts_scr.rearrange("h d t -> d h t"), KTILE.t[:, :, :], r=[KTILE], w=[], dw=D_S)
        else:
            dma('sp', kt_scr[:, :, j * 128:(j + 1) * 128].rearrange("h d t -> d h t"), KTILE.t[:, :, :], r=[KTILE], w=[], dw=D_KT)
        ib = bank()
        proj_feat(A_IK, 64, ib, 0)
        op('act', lambda e: e.copy(out=IKTILE.t[:, :], in_=ib.t[0:64, 0:128]), r=[ib], w=[IKTILE])
        dma('sp', iks_scr if samp else ikt_scr[:, j * 128:(j + 1) * 128], IKTILE.t[:, :], r=[IKTILE], w=[], dw=(D_S if samp else D_IKT))
        if own:
            b4 = proj_tok(A_AK, 512)
            t = tmp()
            op('dve', lambda e, t=t: e.tensor_copy(out=t.t[:, :], in_=b4.t[:, :]), r=[b4], w=[t])
            dma('sp', ks if samp else kp[oi * 128:(oi + 1) * 128, :], t.t[:, :], r=[t], w=[])
            b5 = proj_tok(A_IK, 64)
            op('dve', lambda e: e.tensor_copy(out=IKF.t[:, :], in_=b5.t[:, 0:64]), r=[b5], w=[IKF])
            dma('sp', iks if samp else ikp[oi * 128:(oi + 1) * 128, :], IKF.t[:, :], r=[IKF], w=[])
        if CUT == 3:
            return
        bd = bank()
        op('pe', lambda e: e.matmul(bd.t[:, :], lhsT=CF.t[:, KL:KL + 128], rhs=LOGF.t[:, :], start=True, stop=True), r=[CF, LOGF], w=[bd])
        ebd = tmp()
        op('act', lambda e: e.activation(out=ebd.t[:, :], in_=bd.t[:, :], func=AF.Exp), r=[bd], w=[ebd])
        op('pool', lambda e: e.tensor_tensor(out=KHAT.t[:, :], in0=KK.t[:, :], in1=ebd.t[:, :], op=ALU.mult), r=[KK, ebd], w=[KHAT])
        dc = bank()
        for h in range(4):
            op('pe', lambda e, h=h: e.matmul(dc.t[:, h * NCK:(h + 1) * NCK], lhsT=LOGF.t[:, h * 128:(h + 1) * 128],
                                             rhs=CF.t[:, KIND:KIND + NCK], start=True, stop=True), r=[CF, LOGF], w=[dc])
        op('act', lambda e: e.activation(out=DEC.t[:, 0:4 * NCK], in_=dc.t[:, 0:4 * NCK], func=AF.Exp), r=[dc], w=[DEC])
        if CUT == 11:
            return
        if own:
            bt = bank()
            op('pe', lambda e: e.matmul(bt.t[:, :], lhsT=CF.t[:, KU:KU + 128], rhs=LOGF.t[:, :], start=True, stop=True), r=[CF, LOGF], w=[bt])
            eb = tmp(); enb = tmp()
            op('act', lambda e: e.activation(out=eb.t[:, :], in_=bt.t[:, :], func=AF.Exp), r=[bt], w=[eb])
            op('act', lambda e: e.activation(out=enb.t[:, :], in_=bt.t[:, :], func=AF.Exp, scale=-1.0), r=[bt], w=[enb])
            if CUT == 12:
                return
            hq = proj_tok(A_HQ, 512)
            op('dve', lambda e: e.tensor_tensor(out=QTL.t[:, :], in0=hq.t[:, :], in1=eb.t[:, :], op=ALU.mult), r=[hq, eb], w=[QTL])
            op('pool', lambda e: e.tensor_tensor(out=KTL.t[:, :], in0=KK.t[:, :], in1=enb.t[:, :], op=ALU.mult), r=[KK, enb], w=[KTL])
            if CUT == 13:
                return
            tb = trbank()
            for h in range(4):
                op('pe', lambda e, h=h: e.transpose(out=tb.t[:, h, :], in_=QTL.t[:, h * 128:(h + 1) * 128], identity=IDB), r=[QTL, CB], w=[tb])
            for h in range(4):
                op('pe', lambda e, h=h: e.transpose(out=tb.t[:, 4 + h, :], in_=KTL.t[:, h * 128:(h + 1) * 128], identity=IDB), r=[KTL, CB], w=[tb])
            if CUT == 14:
                return
            op('act', lambda e: e.copy(out=QTT.t[:, :, :], in_=tb.t[:, 0:4, :]), r=[tb], w=[QTT])
            if CUT == 15:
                return
            op('act', lambda e: e.copy(out=KTT.t[:, :, :], in_=tb.t[:, 4:8, :]), r=[tb], w=[KTT])
            if CUT == 7:
                return
            ab = bank()
            for h in range(4):
                op('pe', lambda e, h=h: e.matmul(ab.t[:, h * 128:(h + 1) * 128], lhsT=KTT.t[:, h, :], rhs=QTT.t[:, h, :], start=True, stop=True),
                   r=[KTT, QTT], w=[ab])
            for h in range(4):
                op('dve', lambda e, h=h: e.tensor_tensor(out=AT.t[:, h, :], in0=ab.t[:, h * 128:(h + 1) * 128], in1=CF.t[:, KU:KU + 128], op=ALU.mult),
                   r=[ab, CF], w=[AT])
        if CUT == 4:
            return
        obh = None
        if not samp:
            si = sidx['i']
            s_start, s_mid, s_end = SBF[si % 3], SBF[(si + 1) % 3], SBF[(si + 2) % 3]
            sidx['i'] += 2
            for c, s_out in ((0, s_mid), (1, s_end)):
                op('pool', lambda e, c=c: e.tensor_scalar(out=KHC[c].t[:, :], in0=KHAT.t[:, :], scalar1=CF.t[:, K_IND64 + c:K_IND64 + c + 1], scalar2=None,
                                                          op0=ALU.mult), r=[KHAT, CF], w=[KHC[c]])
                ub = bank()
                for h in range(4):
                    op('pe', lambda e, h=h, c=c, ub=ub: e.matmul(ub.t[:, h * 128:(h + 1) * 128], lhsT=KHC[c].t[:, h * 128:(h + 1) * 128],
                                                         rhs=VB.t[:, h * 128:(h + 1) * 128], start=True, stop=True),
                       r=[KHC[c], VB], w=[ub])
                for h in range(4):
                    op('dve', lambda e, h=h, c=c, ub=ub: e.scalar_tensor_tensor(out=SF.t[:, h, :], in0=SF.t[:, h, :], scalar=DEC.t[:, h * 2 + c:h * 2 + c + 1],
                                                                      in1=ub.t[:, h * 128:(h + 1) * 128], op0=ALU.mult, op1=ALU.add),
                       r=[SF, DEC, ub], w=[SF])
                op('act', lambda e, s_out=s_out: e.copy(out=s_out.t[:, :, :], in_=SF.t[:, :, :]), r=[SF], w=[s_out])
            if CUT == 8:
                return
            if own:
                ob = bank()
                for h in range(4):
                    op('pe', lambda e, h=h: e.matmul(ob.t[:, h * 128:(h + 1) * 128], lhsT=VB.t[:, h * 128:(h + 1) * 128], rhs=AT.t[:, h, :],
                                                     start=True, stop=False), r=[VB, AT], w=[ob])
                    op('pe', lambda e, h=h: e.matmul(ob.t[:, h * 128:h * 128 + 64], lhsT=s_start.t[:, h, :], rhs=QTT.t[:, h, 0:64],
                                                     start=False, stop=False), r=[s_start, QTT], w=[ob])
                    op('pe', lambda e, h=h: e.matmul(ob.t[:, h * 128 + 64:h * 128 + 128], lhsT=s_mid.t[:, h, :], rhs=QTT.t[:, h, 64:128],
                                                     start=False, stop=True), r=[s_mid, QTT], w=[ob])
                obh = [(ob, h * 128) for h in range(4)]
        else:
            obs = [bank() for _ in range(4)]
            st['banks'] = [i for i in range(6) if all(PB[i] is not o_ for o_ in obs)]
            for h in range(4):
                op('pe', lambda e, h=h: e.matmul(obs[h].t[:, 0:128], lhsT=VB.t[:, h * 128:(h + 1) * 128], rhs=AT.t[:, h, :],
                                                 start=True, stop=False), r=[VB, AT], w=[obs[h]])
            for bq in range(NSEQ):
                sf = S0F[bq % 2]; sb_ = S0B[bq % 2]; vm = VM[bq % 2]; sn = SNEW[bq % 2]
                dma('sp', sf.t[:, :, :], st0[bq * 512:(bq + 1) * 512, :].rearrange("(h p) e -> p h e", p=128), r=[], w=[sf])
                op('act', lambda e, sf=sf, sb_=sb_: e.copy(out=sb_.t[:, :, :], in_=sf.t[:, :, :]), r=[sf], w=[sb_])
                for h in range(4):
                    op('pe', lambda e, h=h, bq=bq, sb_=sb_: e.matmul(obs[h].t[:, 4 * bq:4 * bq + 4], lhsT=sb_.t[:, h, :], rhs=QTT.t[:, h, 4 * bq:4 * bq + 4],
                                                                 start=False, stop=(bq == NSEQ - 1)), r=[sb_, QTT], w=[obs[h]])
                op('pool', lambda e, bq=bq, vm=vm: e.tensor_scalar(out=vm.t[:, :], in0=VB.t[:, :], scalar1=CF.t[:, K_IND4 + bq:K_IND4 + bq + 1], scalar2=None,
                                                               op0=ALU.mult), r=[VB, CF], w=[vm])
                ub = bank()
                for h in range(4):
                    op('pe', lambda e, h=h, vm=vm, ub=ub: e.matmul(ub.t[:, h * 128:(h + 1) * 128], lhsT=KHAT.t[:, h * 128:(h + 1) * 128],
                                                           rhs=vm.t[:, h * 128:(h + 1) * 128], start=True, stop=True), r=[KHAT, vm], w=[ub])
                for h in range(4):
                    op('dve', lambda e, h=h, bq=bq, sf=sf, sn=sn, ub=ub: e.scalar_tensor_tensor(
                        out=sn.t[:, h, :], in0=sf.t[:, h, :], scalar=DEC.t[:, h * NCK + bq:h * NCK + bq + 1],
                        in1=ub.t[:, h * 128:(h + 1) * 128], op0=ALU.mult, op1=ALU.add), r=[sf, DEC, ub], w=[sn])
                dma('sp', sts[bq * 512:(bq + 1) * 512, :].rearrange("(h p) e -> p h e", p=128), sn.t[:, :, :], r=[sn], w=[])
            obh = [(obs[h], 0) for h in range(4)]
        if not own:
            return
        if CUT == 9:
            return
        for h in range(4):
            ob_, c0 = obh[h]
            op('act', lambda e, h=h, ob_=ob_, c0=c0: e.activation(out=SQ.t[:, h * 128:(h + 1) * 128], in_=ob_.t[:, c0:c0 + 128], func=AF.Square),
               r=[ob_], w=[SQ])
        ssb = bank()
        op('pe', lambda e: e.matmul(ssb.t[:, :], lhsT=CB.t[:, K_ONES:K_ONES + 128], rhs=SQ.t[:, :], start=True, stop=True), r=[CB, SQ], w=[ssb])
        ro = tmp()
        op('act', lambda e: e.activation(out=ro.t[:, :], in_=ssb.t[:, :], func=AF.Sqrt, scale=1.0 / 128, bias=EPSC), r=[ssb], w=[ro])
        op('dve', lambda e: e.reciprocal(out=ro.t[:, :], in_=ro.t[:, :]), r=[ro], w=[ro])
        gb_ = bank()
        for h in range(4):
            proj_feat(A_HOG + h * 128, 128, gb_, h * 128)
        sil = tmp()
        op('act', lambda e: e.activation(out=sil.t[:, :], in_=gb_.t[:, :], func=AF.Silu), r=[gb_], w=[sil])
        o1 = tmp()
        for h in range(4):
            ob_, c0 = obh[h]
            op('dve', lambda e, h=h, ob_=ob_, c0=c0: e.tensor_tensor(out=o1.t[:, h * 128:(h + 1) * 128], in0=ob_.t[:, c0:c0 + 128],
                                                                 in1=ro.t[:, h * 128:(h + 1) * 128], op=ALU.mult), r=[ob_, ro], w=[o1])
        st['banks'] = list(range(6))
        op('dve', lambda e: e.scalar_tensor_tensor(out=OF.t[:, :, :], in0=o1.t[:, :].rearrange("p (h t) -> p h t", h=4), scalar=HG.t[:, 0:1],
                                                   in1=sil.t[:, :].rearrange("p (h t) -> p h t", h=4), op0=ALU.mult, op1=ALU.mult),
           r=[o1, HG, sil], w=[OF])
        if CUT == 10:
            return
        for half in range(2):
            yb = bank()
            for h in range(4):
                op('pe', lambda e, h=h, half=half, yb=yb: e.matmul(yb.t[:, :], lhsT=OF.t[:, h, :],
                                                           rhs=W.t[:, WA0 + h * 1024 + half * 512:WA0 + h * 1024 + half * 512 + 512],
                                                           start=(h == 0), stop=(h == 3)), r=[OF, W], w=[yb])
            gab = proj_tok(A_GA + half * 512, 512)
            sg = tmp()
            op('act', lambda e, gab=gab, sg=sg: e.activation(out=sg.t[:, :], in_=gab.t[:, :], func=AF.Sigmoid), r=[gab], w=[sg])
            ma = tmp()
            op('dve', lambda e, yb=yb, sg=sg, ma=ma: e.tensor_tensor(out=ma.t[:, :], in0=yb.t[:, :], in1=sg.t[:, :], op=ALU.mult), r=[yb, sg], w=[ma])
            dma('sp', ma_scr[oi * 128:(oi + 1) * 128, half * 512:(half + 1) * 512], ma.t[:, :], r=[ma], w=[], dw=D_MA)

    STOP = int(_ENV.get("STOP", "99"))
    for j in range(min(NT, STOP)):
        X = load_x(xp[j * 128:(j + 1) * 128, :])
        if j % 4 == 3:
            pass1_tile(X, 'own', j, j // 4)
        else:
            pass1_tile(X, 'light', j, None)
    dma('sp', stp.rearrange("(h p) e -> p h e", p=128), SF.t[:, :, :], r=[SF], w=[])
    if STOP > 10:
        X = load_x(xs)
        pass1_tile(X, 'samp', None, NOWN)

    PASSES = int(_ENV.get("PASSES", "3"))
    if PASSES >= 2:
        old = new_pass()
        TP[:] = [carve("q%d" % i, [128, 512], F32) for i in range(5)]
        MK = [carve("mk%d" % i, [128, 512], BF16) for i in range(2)]
        KTC = [carve("ktc%d" % i, [128, 4, 512], BF16) for i in range(2)]
        VC = [carve("vc%d" % i, [128, 4, 512], BF16) for i in range(2)]
        IKC = [carve("ikc%d" % i, [64, 512], BF16) for i in range(2)]
        EX = [carve("ex%d" % i, [128, 512], BF16) for i in range(2)]
        PM = [carve("pm%d" % i, [128, 512], BF16) for i in range(2)]
        PTT = [carve("ptt%d" % i, [128, 4, 128], BF16) for i in range(2)]
        AQT = carve("aqT", [128, 4, 128], BF16); IQT = carve("iqT", [64, 8, 128], BF16)
        IW = carve("iw", [128, 8], F32); IWS = carve("iws", [128, 8], F32); THR = carve("thr", [128, 16], F32)
        NCHR = max(LP // 512, NPG + 1)
        RS = carve("rs", [128, 4 * NCHR], F32)
        AO = carve("ao", [128, 512], BF16); AOT = carve("aoT", [128, 4, 128], BF16)
        MB = carve("mb", [128, 1024], BF16); MT = carve("mT", [128, 8, 128], BF16)
        NPT = NSEQ * NPG
        PTI = carve("pti", [128, NPT], I32); PTF = carve("ptf", [128, NPT], F32); PIDX = carve("pidx", [128, NPT], I32)
        IOTA = carve("iota", [128, 8], I32); IOTF = carve("iotf", [128, 8], F32)
        PGB = [carve("pgb%d" % i, [128, 512], BF16) for i in range(2)]
        IPB = [carve("ipb%d" % i, [128, 64], BF16) for i in range(2)]
        GB = [carve("gb%d" % i, [128, 512], F32) for i in range(4)]
        GI = [carve("gi%d" % i, [128, 64], F32) for i in range(2)]
        fence(old + cur['bufs'] + [W])
        woff = load_w(w2, B_END, [(w_b, 4), (w_o, 8)])
        WB0, WO0 = woff
        IDX_SCALE = 512.0 ** -0.5
        rr = dict(i=0)

        def ring(lst):
            k = id(lst)
            rr[k] = rr.get(k, 0) + 1
            return lst[rr[k] % len(lst)]

        dma('sp', PTI.t[:, :], ptab.partition_broadcast(128), r=[], w=[PTI])
        op('pool', lambda e: e.iota(IOTA.t[:, 0:1], pattern=[[0, 1]], base=0, channel_multiplier=1), r=[], w=[IOTA])
        op('dve', lambda e: e.tensor_copy(out=PTF.t[:, :], in_=PTI.t[:, :]), r=[PTI], w=[PTF])
        op('dve', lambda e: e.tensor_copy(out=IOTF.t[:, 0:1], in_=IOTA.t[:, 0:1]), r=[IOTA], w=[IOTF])
        op('dve', lambda e: e.tensor_scalar(out=PTF.t[:, :], in0=PTF.t[:, :], scalar1=128.0, scalar2=IOTF.t[:, 0:1], op0=ALU.mult, op1=ALU.add),
           r=[PTF, IOTF], w=[PTF])
        op('dve', lambda e: e.tensor_copy(out=PIDX.t[:, :], in_=PTF.t[:, :]), r=[PTF], w=[PIDX])

        class PromptProv:
            def ik(self, c, n):
                b = ring(IKC)
                dma('sp', b.t[0:64, 0:n], ikt_scr[:, c * 512:c * 512 + n], r=[], w=[b], dr=D_IKT)
                return b

            def kv(self, c, n):
                ktc = ring(KTC); vc = ring(VC)
                dma('sp', ktc.t[:, :, 0:n], kt_scr[:, :, c * 512:c * 512 + n].rearrange("h d l -> d h l"), r=[], w=[ktc], dr=D_KT)
                dma('sp', vc.t[:, 0:n // 128, :], v_scr[c * 512:c * 512 + n, :].rearrange("(t p) e -> p t e", p=128), r=[], w=[vc], dr=D_V)
                return ktc, vc

        class SampProv:
            def __init__(self, bq):
                self.bq = bq

            def ik(self, c, n):
                bq = self.bq
                b = ring(IKC)
                nt = n // 128
                npg = sum(1 for tl in range(nt) if 4 * c + tl < NPG)
                tb = trbank() if npg else None
                for tl in range(nt):
                    tile = 4 * c + tl
                    if tile < NPG:
                        pg = ring(GI); ipb = ring(IPB)
                        dma('pool', pg.t[:, 0:64], cik, r=[PIDX], w=[pg], indirect=PIDX.t[:, bq * NPG + tile:bq * NPG + tile + 1])
                        op('act', lambda e, pg=pg, ipb=ipb: e.copy(out=ipb.t[:, :], in_=pg.t[:, 0:64]), r=[pg], w=[ipb])
                        op('pe', lambda e, tb=tb, ipb=ipb, tl=tl: e.transpose(out=tb.t[0:64, tl, :], in_=ipb.t[:, 0:64], identity=IDB), r=[ipb, CB], w=[tb])
                    else:
                        op('pool', lambda e, b=b, tl=tl: e.memset(b.t[0:64, tl * 128:(tl + 1) * 128], 0.0), r=[], w=[b])
                        dma('sp', b.t[0:64, tl * 128:tl * 128 + 4], iks_scr[:, 4 * bq:4 * bq + 4], r=[], w=[b], dr=D_S)
                if npg:
                    op('act', lambda e, tb=tb, b=b, npg=npg: e.copy(out=b.t[0:64, 0:npg * 128], in_=tb.t[0:64, 0:npg, :]), r=[tb], w=[b])
                return b

            def kv(self, c, n):
                bq = self.bq
                ktc = ring(KTC); vc = ring(VC)
                nt = n // 128
                for tl in range(nt):
                    tile = 4 * c + tl
                    if tile < NPG:
                        col = PIDX.t[:, bq * NPG + tile:bq * NPG + tile + 1]
                        pg = ring(GB)
                        dma('pool', pg.t[:, :], ck, r=[PIDX], w=[pg], indirect=col)
                        pgb = ring(PGB)
                        op('act', lambda e, pg=pg, pgb=pgb: e.copy(out=pgb.t[:, :], in_=pg.t[:, :]), r=[pg], w=[pgb])
                        tb = trbank()
                        for h in range(4):
                            op('pe', lambda e, h=h, tb=tb, pgb=pgb: e.transpose(out=tb.t[:, h, :], in_=pgb.t[:, h * 128:(h + 1) * 128], identity=IDB),
                               r=[pgb, CB], w=[tb])
                        op('act', lambda e, tb=tb, ktc=ktc, tl=tl: e.copy(out=ktc.t[:, :, tl * 128:(tl + 1) * 128], in_=tb.t[:, 0:4, :]), r=[tb], w=[ktc])
                        pg2 = ring(GB)
                        dma('pool', pg2.t[:, :], cv, r=[PIDX], w=[pg2], indirect=col)
                        op('dve', lambda e, pg2=pg2, vc=vc, tl=tl: e.tensor_copy(out=vc.t[:, tl, :], in_=pg2.t[:, :]), r=[pg2], w=[vc])
                    else:
                        op('pool', lambda e, ktc=ktc, tl=tl: e.memset(ktc.t[:, :, tl * 128:(tl + 1) * 128], 0.0), r=[], w=[ktc])
                        dma('sp', ktc.t[:, :, tl * 128:tl * 128 + 4], kts_scr[:, :, 4 * bq:4 * bq + 4].rearrange("h d t -> d h t"), r=[], w=[ktc], dr=D_S)
                        op('pool', lambda e, vc=vc, tl=tl: e.memset(vc.t[:, tl, :], 0.0), r=[], w=[vc])
                        dma('sp', vc.t[0:4, tl, :], vs_scr[4 * bq:4 * bq + 4, :], r=[], w=[vc], dr=D_S)
                return ktc, vc

        SCb = U1

        def attend(nq, qc0, iwb, L, ksel, prov, CH, prompt):
            SC = U1.t
            nch = (L + CH - 1) // CH
            IDQ = CB.t[0:nq, K_ID:K_ID + nq]
            for c in range(nch):
                n = min(CH, L - c * CH)
                ikc = prov.ik(c, n)
                for h in range(8):
                    b = bank()
                    op('pe', lambda e, b=b, h=h, ikc=ikc, n=n: e.matmul(b.t[0:nq, 0:n], lhsT=IQT.t[0:64, h, qc0:qc0 + nq], rhs=ikc.t[0:64, 0:n],
                                                                       start=True, stop=True), r=[IQT, ikc], w=[b])
                    rl = tmp()
                    op('act', lambda e, b=b, rl=rl, n=n: e.activation(out=rl.t[0:nq, 0:n], in_=b.t[0:nq, 0:n], func=AF.Relu), r=[b], w=[rl])
                    if h == 0:
                        op('dve', lambda e, rl=rl, c=c, n=n: e.tensor_scalar(out=SC[0:nq, c * CH:c * CH + n], in0=rl.t[0:nq, 0:n], scalar1=iwb.t[0:nq, 0:1],
                                                                          scalar2=None, op0=ALU.mult), r=[rl, iwb], w=[SCb])
                    else:
                        op('dve', lambda e, rl=rl, c=c, n=n, h=h: e.scalar_tensor_tensor(out=SC[0:nq, c * CH:c * CH + n], in0=rl.t[0:nq, 0:n],
                                                                                      scalar=iwb.t[0:nq, h:h + 1], in1=SC[0:nq, c * CH:c * CH + n],
                                                                                      op0=ALU.mult, op1=ALU.add), r=[rl, iwb, SCb], w=[SCb])
            T = THR.t
            op('dve', lambda e: e.tensor_reduce(out=T[0:nq, 0:1], in_=SC[0:nq, 0:L], axis=AX.X, op=ALU.max), r=[SCb], w=[THR])
            op('dve', lambda e: e.tensor_reduce(out=T[0:nq, 1:2], in_=SC[0:nq, 0:L], axis=AX.X, op=ALU.min), r=[SCb], w=[THR])
            if prompt:
                for t in range(3):
                    op('dve', lambda e, t=t: e.tensor_scalar(out=SC[0:nq, t * 128:(t + 1) * 128], in0=SC[0:nq, t * 128:(t + 1) * 128],
                                                            scalar1=PADB.t[0:nq, t:t + 1], scalar2=None, op0=ALU.add), r=[SCb, PADB], w=[SCb])
            op('dve', lambda e: e.tensor_tensor(out=SC[0:nq, L - 128:L], in0=SC[0:nq, L - 128:L], in1=CF.t[0:nq, K_CNEG:K_CNEG + 128], op=ALU.add),
               r=[SCb, CF], w=[SCb])
            op('dve', lambda e: e.tensor_tensor(out=T[0:nq, 2:3], in0=T[0:nq, 0:1], in1=T[0:nq, 1:2], op=ALU.subtract), r=[THR], w=[THR])
            op('dve', lambda e: e.tensor_copy(out=T[0:nq, 3:4], in_=T[0:nq, 1:2]), r=[THR], w=[THR])
            for it in range(1, NIT + 1):
                sc = 2.0 ** -it
                op('dve', lambda e, sc=sc: e.scalar_tensor_tensor(out=T[0:nq, 4:5], in0=T[0:nq, 2:3], scalar=sc, in1=T[0:nq, 3:4], op0=ALU.mult, op1=ALU.add),
                   r=[THR], w=[THR])
                op('dve', lambda e: e.tensor_scalar(out=JK.t[0:nq, 0:L], in0=SC[0:nq, 0:L], scalar1=T[0:nq, 4:5], scalar2=None, op0=ALU.is_ge, op1=ALU.add,
                                                    accum_out=T[0:nq, 5:6]), r=[SCb, THR], w=[JK, THR])
                op('dve', lambda e: e.tensor_scalar(out=T[0:nq, 6:7], in0=T[0:nq, 5:6], scalar1=float(ksel) - 0.5, scalar2=None, op0=ALU.is_ge),
                   r=[THR], w=[THR])
                op('dve', lambda e: e.tensor_tensor(out=T[0:nq, 7:8], in0=T[0:nq, 6:7], in1=T[0:nq, 2:3], op=ALU.mult), r=[THR], w=[THR])
                op('dve', lambda e, sc=sc: e.scalar_tensor_tensor(out=T[0:nq, 3:4], in0=T[0:nq, 7:8], scalar=sc, in1=T[0:nq, 3:4], op0=ALU.mult, op1=ALU.add),
                   r=[THR], w=[THR])
            st['banks'] = [4, 5]
            OA = PB[0:4]
            op('pool', lambda e: e.memset(RS.t[:, :], 0.0), r=[], w=[RS])
            for c in range(nch):
                n = min(CH, L - c * CH)
                nt = n // 128
                ktc, vc = prov.kv(c, n)
                mk = ring(MK)
                op('dve', lambda e, mk=mk, c=c, n=n: e.tensor_scalar(out=mk.t[0:nq, 0:n], in0=SC[0:nq, c * CH:c * CH + n], scalar1=T[0:nq, 3:4], scalar2=None,
                                                                  op0=ALU.is_ge), r=[SCb, THR], w=[mk])
                for h in range(4):
                    lg = bank()
                    op('pe', lambda e, lg=lg, h=h, ktc=ktc, n=n: e.matmul(lg.t[0:nq, 0:n], lhsT=AQT.t[:, h, qc0:qc0 + nq], rhs=ktc.t[:, h, 0:n],
                                                                         start=True, stop=True), r=[AQT, ktc], w=[lg])
                    ex = ring(EX); pm = ring(PM); pt = ring(PTT)
                    op('act', lambda e, lg=lg, ex=ex, n=n: e.activation(out=ex.t[0:nq, 0:n], in_=lg.t[0:nq, 0:n], func=AF.Exp), r=[lg], w=[ex])
                    op('dve', lambda e, ex=ex, pm=pm, mk=mk, n=n, c=c, h=h: e.scalar_tensor_tensor(
                        out=pm.t[0:nq, 0:n], in0=ex.t[0:nq, 0:n], scalar=1.0, in1=mk.t[0:nq, 0:n], op0=ALU.mult, op1=ALU.mult,
                        accum_out=RS.t[0:nq, h * NCHR + c:h * NCHR + c + 1]), r=[ex, mk], w=[pm, RS])
                    tb = trbank()
                    for t in range(nt):
                        op('pe', lambda e, tb=tb, pm=pm, t=t: e.transpose(out=tb.t[:, t, 0:nq], in_=pm.t[0:nq, t * 128:(t + 1) * 128], identity=IDQ),
                           r=[pm, CB], w=[tb])
                    op('act', lambda e, tb=tb, pt=pt, nt=nt: e.copy(out=pt.t[:, 0:nt, 0:nq], in_=tb.t[:, 0:nt, 0:nq]), r=[tb], w=[pt])
                    for t in range(nt):
                        op('pe', lambda e, pt=pt, vc=vc, t=t, h=h, c=c, nt=nt: e.matmul(
                            OA[h].t[0:nq, 0:128], lhsT=pt.t[:, t, 0:nq], rhs=vc.t[:, t, h * 128:(h + 1) * 128],
                            start=(c == 0 and t == 0), stop=(c == nch - 1 and t == nt - 1)), r=[pt, vc], w=[OA[h]])
            if _ENV.get("DBG") == "1" and not prompt and qc0 == 0:
                dma('sp', ks[0:4, 0:L], U1.t[0:4, 0:L], r=[U1], w=[])
                dma('sp', ks[4:8, 0:16], THR.t[0:4, 0:16], r=[THR], w=[])
                dma('sp', ks[8:12, 0:4 * nch], RS.t[0:4, 0:4 * nch], r=[RS], w=[])
            op('dve', lambda e: e.tensor_reduce(out=T[0:nq, 8:12], in_=RS.t[0:nq, 0:4 * NCHR].rearrange("p (h c) -> p h c", h=4), axis=AX.X, op=ALU.add),
               r=[RS], w=[THR])
            op('dve', lambda e: e.reciprocal(out=T[0:nq, 12:16], in_=T[0:nq, 8:12]), r=[THR], w=[THR])
            for h in range(4):
                op('act', lambda e, h=h: e.activation(out=AO.t[0:nq, h * 128:(h + 1) * 128], in_=OA[h].t[0:nq, 0:128], func=AF.Copy,
                                                      scale=T[0:nq, 12 + h:13 + h]), r=[OA[h], THR], w=[AO])
            tb = trbank()
            for h in range(4):
                op('pe', lambda e, tb=tb, h=h: e.transpose(out=tb.t[:, h, 0:nq], in_=AO.t[0:nq, h * 128:(h + 1) * 128], identity=IDQ), r=[AO, CB], w=[tb])
            op('act', lambda e, tb=tb: e.copy(out=AOT.t[:, :, qc0:qc0 + nq], in_=tb.t[:, 0:4, 0:nq]), r=[tb], w=[AOT])
            st['banks'] = list(range(6))

        def pass2_tile(X, oi, samp):
            to_hT(X, G1)
            ab = bank()
            for h in range(4):
                proj_feat(B_AQ + h * 128, 128, ab, h * 128)
            op('act', lambda e: e.activation(out=AQT.t[:, :, :], in_=ab.t[:, :].rearrange("p (h t) -> p h t", h=4), func=AF.Copy, scale=128.0 ** -0.5),
               r=[ab], w=[AQT])
            for g in range(2):
                ibk = bank()
                for hh in range(4):
                    proj_feat(B_IQ + (g * 4 + hh) * 64, 64, ibk, hh * 128)
                op('act', lambda e, g=g, ibk=ibk: e.copy(out=IQT.t[0:64, g * 4:(g + 1) * 4, :], in_=ibk.t[0:64, :].rearrange("p (h t) -> p h t", h=4)),
                   r=[ibk], w=[IQT])
            wb_ = proj_tok(B_IW, 8)
            op('dve', lambda e: e.tensor_scalar(out=IW.t[:, :], in0=wb_.t[:, 0:8], scalar1=IDX_SCALE, scalar2=None, op0=ALU.mult), r=[wb_], w=[IW])
            if not samp:
                attend(128, 0, IW, (4 * oi + 4) * 128, KSEL_P, PromptProv(), 512, True)
            else:
                op('pool', lambda e: e.memset(AOT.t[:, :, :], 0.0), r=[], w=[AOT])
                for bq in range(NSEQ):
                    dma('sp', IWS.t[0:4, :], IW.t[4 * bq:4 * bq + 4, :], r=[IW], w=[IWS])
                    attend(4, 4 * bq, IWS, LS, KSEL_S, SampProv(bq), 512, False)
            if _ENV.get("DBG") == "2" and samp:
                dma('pool', ks[:, :].rearrange("p (h t) -> p h t", h=4), AOT.t[:, :, :], r=[AOT], w=[])
            for half in range(2):
                yb = bank()
                for h in range(4):
                    op('pe', lambda e, h=h, half=half, yb=yb: e.matmul(yb.t[:, :], lhsT=AOT.t[:, h, :],
                                                                      rhs=W.t[:, WB0 + h * 1024 + half * 512:WB0 + h * 1024 + half * 512 + 512],
                                                                      start=(h == 0), stop=(h == 3)), r=[AOT, W], w=[yb])
                gbb = proj_tok(B_GB + half * 512, 512)
                sg = tmp(); m = tmp(); mat = tmp()
                op('act', lambda e, gbb=gbb, sg=sg: e.activation(out=sg.t[:, :], in_=gbb.t[:, :], func=AF.Sigmoid), r=[gbb], w=[sg])
                op('dve', lambda e, yb=yb, sg=sg, m=m: e.tensor_tensor(out=m.t[:, :], in0=yb.t[:, :], in1=sg.t[:, :], op=ALU.mult), r=[yb, sg], w=[m])
                dma('sp', mat.t[:, :], ma_scr[oi * 128:(oi + 1) * 128, half * 512:(half + 1) * 512], r=[], w=[mat], dr=D_MA)
                op('pool', lambda e, m=m, mat=mat, half=half: e.tensor_tensor(out=MB.t[:, half * 512:(half + 1) * 512], in0=m.t[:, :], in1=mat.t[:, :], op=ALU.add),
                   r=[m, mat], w=[MB])
            if _ENV.get("DBG") == "3" and samp:
                dma('pool', ks[:, :], MB.t[:, 0:512], r=[MB], w=[])
                dma('pool', vs[:, :], MB.t[:, 512:1024], r=[MB], w=[])
            tb = trbank()
            for k in range(8):
                op('pe', lambda e, k=k, tb=tb: e.transpose(out=tb.t[:, k, :], in_=MB.t[:, k * 128:(k + 1) * 128], identity=IDB), r=[MB, CB], w=[tb])
            op('act', lambda e, tb=tb: e.copy(out=MT.t[:, :, :], in_=tb.t[:, :, :]), r=[tb], w=[MT])
            for half in range(2):
                xb = bank()
                for k in range(8):
                    op('pe', lambda e, k=k, half=half, xb=xb: e.matmul(xb.t[:, :], lhsT=MT.t[:, k, :],
                                                                      rhs=W.t[:, WO0 + k * 1024 + half * 512:WO0 + k * 1024 + half * 512 + 512],
                                                                      start=(k == 0), stop=(k == 7)), r=[MT, W], w=[xb])
                x1 = tmp()
                op('dve', lambda e, xb=xb, x1=x1, half=half, X=X: e.tensor_tensor(out=x1.t[:, :], in0=xb.t[:, :], in1=X.t[:, half * 512:(half + 1) * 512], op=ALU.add),
                   r=[xb, X], w=[x1])
                dma('sp', x1_scr[oi * 128:(oi + 1) * 128, half * 512:(half + 1) * 512], x1.t[:, :], r=[x1], w=[], dw=D_X1)
                if PASSES == 2:
                    dst = ys if samp else yp[oi * 128:(oi + 1) * 128, :]
                    dma('sp', dst[:, half * 512:(half + 1) * 512], x1.t[:, :], r=[x1], w=[])

        for oi in range(NOWN):
            X = load_x(xp[(4 * oi + 3) * 128:(4 * oi + 4) * 128, :])
            pass2_tile(X, oi, False)
        X = load_x(xs)
        pass2_tile(X, NOWN, True)

    if PASSES >= 3:
        old = new_pass()
        TP[:] = [carve("r%d" % i, [128, 512], F32) for i in range(4)]
        X1T = [carve("x1t%d" % i, [128, 1024], F32) for i in range(4)]
        H2T = carve("h2T", [128, 8, 512], BF16)
        X2 = [carve("x2_%d" % i, [128, 1024], F32) for i in range(2)]
        YT = [carve("yt%d" % i, [128, 1024], F32) for i in range(2)]
        HID = U1.t[:, 0:8192].bitcast(BF16).rearrange("p (c t) -> p c t", c=32)
        fence(old + cur['bufs'] + [W, U1, G1, LB, OML, G2, GF])
        dma('sp', G2.t[:, :], n2.partition_broadcast(128), r=[], w=[G2])
        dma('sp', GF.t[:, :], nf.partition_broadcast(128), r=[], w=[GF])
        NTL = NOWN + 1
        GS = int(_ENV.get("GS", "4"))
        for g0 in range(0, NTL, GS):
            tiles = list(range(g0, min(NTL, g0 + GS)))
            ntok = len(tiles) * 128
            for ti, t in enumerate(tiles):
                Xt = X1T[ti]
                dma('sp', Xt.t[:, :], x1_scr[t * 128:(t + 1) * 128, :], r=[], w=[Xt], dr=D_X1)
                rstd = rmsnorm_rstd(Xt)
                op('dve', lambda e, Xt=Xt, rstd=rstd: e.scalar_tensor_tensor(out=H.t[:, :], in0=Xt.t[:, :], scalar=rstd, in1=G2.t[:, :], op0=ALU.mult, op1=ALU.mult),
                   r=[Xt, SS, G2], w=[H])
                tb = trbank()
                for k in range(8):
                    op('pe', lambda e, k=k, tb=tb: e.transpose(out=tb.t[:, k, :], in_=H.t[:, k * 128:(k + 1) * 128], identity=IDB), r=[H, CB], w=[tb])
                op('act', lambda e, tb=tb, ti=ti: e.copy(out=H2T.t[:, :, ti * 128:(ti + 1) * 128], in_=tb.t[:, :, :]), r=[tb], w=[H2T])
            for k in range(8):
                for c0 in range(0, 4096, 2048):
                    dma('pool', W.t[:, k * 4096 + c0:k * 4096 + c0 + 2048], w_up[k * 128:(k + 1) * 128, c0:c0 + 2048], r=[], w=[W])
            for c in range(32):
                hb = bank()
                for k in range(8):
                    op('pe', lambda e, k=k, c=c, hb=hb, ntok=ntok: e.matmul(hb.t[:, 0:ntok], lhsT=W.t[:, k * 4096 + c * 128:k * 4096 + (c + 1) * 128], rhs=H2T.t[:, k, 0:ntok],
                                                              start=(k == 0), stop=(k == 7)), r=[W, H2T], w=[hb])
                rl = tmp()
                op('act', lambda e, hb=hb, rl=rl, ntok=ntok: e.activation(out=rl.t[:, 0:ntok], in_=hb.t[:, 0:ntok], func=AF.Relu), r=[hb], w=[rl])
                op('pool', lambda e, rl=rl, c=c, ntok=ntok: e.tensor_tensor(out=HID[:, c, 0:ntok], in0=rl.t[:, 0:ntok], in1=rl.t[:, 0:ntok], op=ALU.mult), r=[rl], w=[U1])
                if _ENV.get("DBG") == "6" and c == 0 and g0 == 0:
                    dma('sp', ks[:, 0:ntok], rl.t[:, 0:ntok], r=[rl], w=[])
                    dma('pool', vs[:, 0:ntok], HID[:, 0, 0:ntok], r=[U1], w=[])
                    dma('pool', iks[:, 0:64], H2T.t[:, 0, 192:256], r=[H2T], w=[])
            for c in range(32):
                dma('pool', W.t[:, c * 1024:(c + 1) * 1024], w_dn[c * 128:(c + 1) * 128, :], r=[], w=[W])
            for ti, t in enumerate(tiles):
                Xt = X1T[ti]
                x2 = X2[ti % 2]; yt = YT[ti % 2]
                for half in range(2):
                    yb = bank()
                    for c in range(32):
                        op('pe', lambda e, c=c, half=half, yb=yb, ti=ti: e.matmul(yb.t[:, :], lhsT=HID[:, c, ti * 128:(ti + 1) * 128],
                                                                             rhs=W.t[:, c * 1024 + half * 512:c * 1024 + half * 512 + 512],
                                                                             start=(c == 0), stop=(c == 31)), r=[U1, W], w=[yb])
                    op('dve', lambda e, yb=yb, x2=x2, Xt=Xt, half=half: e.tensor_tensor(out=x2.t[:, half * 512:(half + 1) * 512], in0=yb.t[:, :],
                                                                                   in1=Xt.t[:, half * 512:(half + 1) * 512], op=ALU.add), r=[yb, Xt], w=[x2])
                rstd = rmsnorm_rstd(x2)
                op('dve', lambda e, x2=x2, yt=yt, rstd=rstd: e.scalar_tensor_tensor(out=yt.t[:, :], in0=x2.t[:, :], scalar=rstd, in1=GF.t[:, :], op0=ALU.mult, op1=ALU.mult),
                   r=[x2, SS, GF], w=[yt])
                DBGV = _ENV.get("DBG")
                src = {"4": Xt, "5": x2}.get(DBGV, yt)
                dma('sp', ys if t == NOWN else yp[t * 128:(t + 1) * 128, :], src.t[:, :], r=[src], w=[])

    P.finish()
    es.close()
    return nc


FULL_CFG = dict(NT=64, NSEQ=16, NPG=16, NPHYS=2560, KSEL_P=256, KSEL_S=256)
_NC_CACHE = {}


def _prep_core(c, cfg, inp):
    NT, NSEQ, NPG, NPHYS = cfg['NT'], cfg['NSEQ'], cfg['NPG'], cfg['NPHYS']
    s, r = c // 4, c % 4
    pad = 3 - r
    f32 = np.float32
    xp = np.zeros((NT * 128, 1024), f32)
    xp[pad * 128:] = inp['x_prompt'][s, :(NT - pad) * 128]
    padb = np.zeros((128, 4), f32)
    padb[:, :pad] = -BIG
    xs = np.zeros((128, 1024), f32)
    xs[:4 * NSEQ] = inp['x_sample'][c * NSEQ:(c + 1) * NSEQ].reshape(4 * NSEQ, 1024)
    wi = inp['w_in'][0]
    col = lambda name, n: wi[:, R_OFF[name]:R_OFF[name] + n]
    w1 = np.concatenate([col('hf', 512), col('hi', 512), col('av', 512), col('ak', 512), col('ik', 64),
                         col('hq', 512), col('hog', 512), col('ga', 1024)], axis=1)
    w2 = np.concatenate([col('aq', 512), col('iq', 512), col('iw', 8), col('gb', 1024)], axis=1)
    d = dict(
        xp=xp, xs=xs, padb=padb,
        st0=inp['state_hgrn'][0, c * NSEQ:(c + 1) * NSEQ].reshape(NSEQ * 512, 128),
        ptab=inp['page_table'][c * NSEQ:(c + 1) * NSEQ].reshape(1, NSEQ * NPG).astype(np.int32),
        ck=inp['cache_k'][0].reshape(NPHYS * 128, 512), cv=inp['cache_v'][0].reshape(NPHYS * 128, 512),
        cik=inp['cache_idx_k'][0].reshape(NPHYS * 128, 64),
        cst=make_consts(), w1=w1, w2=w2, w_a=inp['w_a'][0], w_b=inp['w_b'][0], w_o=inp['w_o'][0],
        w_up=inp['w_up'][0], w_dn=inp['w_down'][0], lbl=inp['lb_logits'], hgn=inp['hg_norm'][0].reshape(128, 1),
        n1=inp['norm1'][0].reshape(1, 1024), n2=inp['norm2'][0].reshape(1, 1024), nf=inp['norm_f'].reshape(1, 1024))
    return {k: np.ascontiguousarray(v) for k, v in d.items()}


def run_cfg(cfg, inp, names=None):
    key = tuple(sorted(cfg.items()))
    if key not in _NC_CACHE:
        _NC_CACHE[key] = build(cfg)
    nc = _NC_CACHE[key]
    cores = cfg.get('cores', tuple(range(8)))
    in_maps = [_prep_core(c, cfg, inp) for c in cores]
    res = run_bass_kernel_spmd(nc, in_maps, core_ids=list(range(len(cores))))
    out = [None] * 8
    for i, c in enumerate(cores):
        out[c] = res.results[i]
    return out


def assemble(cfg, res):
    NT, NSEQ = cfg['NT'], cfg['NSEQ']
    NOWN = NT // 4
    SEQ = NT * 128
    f32 = np.float32
    y_p = np.zeros((2, SEQ, 1024), f32); k_p = np.zeros((1, 2, SEQ, 4, 128), f32); v_p = np.zeros((1, 2, SEQ, 4, 128), f32)
    i_p = np.zeros((1, 2, SEQ, 64), f32); s_p = np.zeros((1, 2, 4, 128, 128), f32)
    nb = 8 * NSEQ
    y_s = np.zeros((nb, 4, 1024), f32); k_s = np.zeros((1, nb, 4, 4, 128), f32); v_s = np.zeros((1, nb, 4, 4, 128), f32)
    i_s = np.zeros((1, nb, 4, 64), f32); s_s = np.zeros((1, nb, 4, 128, 128), f32)
    for c in range(8):
        s, r = c // 4, c % 4
        o = res[c]
        if o is None:
            continue
        for oi in range(NOWN):
            j = 4 * oi + r
            sl = slice(j * 128, (j + 1) * 128)
            so = slice(oi * 128, (oi + 1) * 128)
            if 'yp' in o:
                y_p[s, sl] = o['yp'][so]
            k_p[0, s, sl] = o['kp'][so].reshape(128, 4, 128)
            v_p[0, s, sl] = o['vp'][so].reshape(128, 4, 128)
            i_p[0, s, sl] = o['ikp'][so]
        if r == 3:
            s_p[0, s] = o['stp'].reshape(4, 128, 128)
        bs = slice(c * NSEQ, (c + 1) * NSEQ)
        if 'ys' in o:
            y_s[bs] = o['ys'][:4 * NSEQ].reshape(NSEQ, 4, 1024)
        k_s[0, bs] = o['ks'][:4 * NSEQ].reshape(NSEQ, 4, 4, 128)
        v_s[0, bs] = o['vs'][:4 * NSEQ].reshape(NSEQ, 4, 4, 128)
        i_s[0, bs] = o['iks'][:4 * NSEQ].reshape(NSEQ, 4, 64)
        s_s[0, bs] = o['sts'].reshape(NSEQ, 4, 128, 128)
    return (y_p, y_s, k_p, v_p, i_p, s_p, k_s, v_s, i_s, s_s)


def kernel(**inputs):
    inp = {k: np.asarray(v) for k, v in inputs.items()}
    res = run_cfg(FULL_CFG, inp)
    return assemble(FULL_CFG, res)
```

```python
import os
import numpy as np
from contextlib import ExitStack
import concourse.bass as bass
import concourse.mybir as mybir
from concourse.bass_utils import run_bass_kernel_spmd

F32 = mybir.dt.float32
BF16 = mybir.dt.bfloat16
I32 = mybir.dt.int32
AF = mybir.ActivationFunctionType
ALU = mybir.AluOpType
AX = mybir.AxisListType

ENGS = ('pe', 'act', 'dve', 'pool', 'sp')
_ENV = os.environ if os.environ.get('MK_DEBUG') else {}


class Buf:
    def __init__(self, name, t):
        self.name = name
        self.t = t
        self.w = None
        self.r = []
        self.dcnt = 0
        self.stores = []


class Prog:
    def __init__(self, nc, es):
        self.nc = nc
        self.es = es
        self.ops = {e: [] for e in ENGS}
        self.cnt = {e: 0 for e in ENGS}
        self.seen = {e: {} for e in ENGS}
        self.dsems = {}
        self.nbuf = 0
        self.cbuf = None
        self.cvals = {}

    def sb(self, name, shape, dtype):
        t = self.es.enter_context(self.nc.sbuf_tensor(name, list(shape), dtype))
        return Buf(name, t)

    def ps(self, name, shape, dtype):
        t = self.es.enter_context(self.nc.psum_tensor(name, list(shape), dtype))
        return Buf(name, t)

    def view(self, name, t):
        return Buf(name, t)

    def const_col(self, val):
        if self.cbuf is None:
            self.cbuf = self.sb("constcols", [128, 32], F32)
        if val not in self.cvals:
            i = len(self.cvals)
            assert i < 32
            self.cvals[val] = i
            self.op('pool', lambda e, i=i, val=val: e.memset(self.cbuf.t[:, i:i + 1], float(val)), r=[], w=[self.cbuf])
        i = self.cvals[val]
        return self.cbuf.t[:, i:i + 1]

    def _deps(self, eng, r, w):
        deps = {}
        def add(tok):
            if tok is None:
                return
            k, v = tok
            if k == eng and eng == 'pe':
                return
            if deps.get(k, 0) < v:
                deps[k] = v
        for b in r:
            add(b.w)
        for b in w:
            add(b.w)
            for tok in b.r:
                add(tok)
        waits = []
        seen = self.seen[eng]
        for k, v in deps.items():
            if seen.get(k, 0) >= v:
                continue
            seen[k] = v
            waits.append((k, v))
        return waits

    def _commit(self, tok, r, w):
        ws = set(id(b) for b in w)
        for b in w:
            b.w = tok
            b.r = []
        for b in r:
            if id(b) not in ws:
                b.r.append(tok)

    def op(self, eng, fn, r, w):
        if self.cbuf is not None and self.cbuf not in r and self.cbuf not in w:
            r = list(r) + [self.cbuf]
        waits = self._deps(eng, r, w)
        self.cnt[eng] += 1
        tok = (eng, self.cnt[eng])
        self.ops[eng].append((waits, fn, (eng, 1)))
        self._commit(tok, r, w)

    def dma(self, q, out_ap, in_ap, r, w, indirect=None, sem=None, dw=None, dr=None):
        sb = sem if sem is not None else (w[0] if w else r[0])
        waits = self._deps(q, r, w)
        if dr is not None:
            seen = self.seen[q]
            for (k, v) in dr.stores:
                if seen.get(k, 0) < v:
                    seen[k] = v
                    waits = [x for x in waits if x[0] != k] + [(k, v)]
        sb.dcnt += 1
        key = ('d', sb.name)
        self.dsems[sb.name] = sb
        tok = (key, 16 * sb.dcnt)
        if indirect is None:
            fn = lambda e: e.dma_start(out=out_ap, in_=in_ap)
        else:
            fn = lambda e: e.indirect_dma_start(out=out_ap, out_offset=None, in_=in_ap,
                                                in_offset=bass.IndirectOffsetOnAxis(ap=indirect, axis=0))
        self.ops[q].append((waits, fn, (key, 16)))
        self._commit(tok, r, w)
        if dw is not None:
            dw.stores.append(tok)

    def finish(self):
        nc = self.nc
        sem = {}
        for e in ENGS:
            sem[e] = self.es.enter_context(nc.semaphore("s_" + e))
        for name in self.dsems:
            sem[('d', name)] = self.es.enter_context(nc.semaphore("d_" + name))
        final = [(('d', n), 16 * b.dcnt) for n, b in self.dsems.items()]
        final += [(e, self.cnt[e]) for e in ENGS if e != 'sp' and self.cnt[e] > 0]

        def replay(ename, e):
            for waits, fn, inc in self.ops[ename]:
                for k, v in waits:
                    e.wait_ge(sem[k], v)
                ins = fn(e)
                ins.then_inc(sem[inc[0]], inc[1])
            if ename == 'sp':
                for k, v in final:
                    e.wait_ge(sem[k], v)

        with nc.Block() as block:
            @block.tensor
            def _(e):
                replay('pe', e)

            @block.scalar
            def _(e):
                replay('act', e)

            @block.vector
            def _(e):
                replay('dve', e)

            @block.gpsimd
            def _(e):
                replay('pool', e)

            @block.sync
            def _(e):
                replay('sp', e)


C_HF, C_HI, C_AV, C_AK, C_IK, C_HQ, C_HOG, C_AQ, C_IQ, C_IW, C_GA, C_GB, C_END = (
    0, 512, 1024, 1536, 2048, 2112, 2624, 3136, 3648, 4160, 4168, 5192, 6216)
R_OFF = dict(hq=0, hf=512, hi=1024, hog=1536, aq=2048, ak=2560, av=3072, iq=3584, ik=4096, iw=4160,
             ga=4168, gb=5192)
BIG = 1.0e30
EPS = 1e-6
K_ID, K_U64, K_L64, K_U4, K_L4, K_CNEG, K_IND64, K_IND4, K_ONES, K_END = (
    0, 128, 256, 384, 512, 640, 768, 770, 802, 930)


def make_consts():
    c = np.zeros((128, 1024), np.float32)
    p = np.arange(128)[:, None]
    j = np.arange(128)[None, :]
    c[:, K_ID:K_ID + 128] = (p == j)
    for (ku, kl, C) in ((K_U64, K_L64, 64), (K_U4, K_L4, 4)):
        same = (p // C) == (j // C)
        c[:, ku:ku + 128] = same & (p <= j)
        c[:, kl:kl + 128] = same & (p > j)
    c[:, K_CNEG:K_CNEG + 128] = np.where(j <= p, 0.0, -BIG)
    c[:, K_IND64:K_IND64 + 2] = (p // 64) == np.arange(2)[None, :]
    c[:, K_IND4:K_IND4 + 32] = (p // 4) == np.arange(32)[None, :]
    c[:, K_ONES:K_ONES + 128] = 1.0
    return c


A_HF, A_HI, A_AV, A_AK, A_IK, A_HQ, A_HOG, A_GA, A_END = 0, 512, 1024, 1536, 2048, 2112, 2624, 3136, 4160
B_AQ, B_IQ, B_IW, B_GB, B_END = 0, 512, 1024, 1032, 2056
ARENA = 8 * A_END + 4 * 1024


def build(cfg):
    NT, NSEQ, NPG, NPHYS = cfg['NT'], cfg['NSEQ'], cfg['NPG'], cfg['NPHYS']
    KSEL_P, KSEL_S = cfg['KSEL_P'], cfg['KSEL_S']
    NIT = cfg.get('NIT', 13)
    NOWN = NT // 4
    LP = NT * 128
    LS = (NPG + 1) * 128
    LMAX = max(LP, LS)
    NCH = LMAX // 512 + 1
    nc = bass.Bass("TRN2", target_bir_lowering=False)

    def din(name, shape, dt=F32):
        return nc.dram_tensor(name, list(shape), dt, kind="ExternalInput").ap()

    def dout(name, shape, dt=F32):
        return nc.dram_tensor(name, list(shape), dt, kind="ExternalOutput").ap()

    def dscr(name, shape, dt):
        return nc.dram_tensor(name, list(shape), dt, kind="Internal").ap()

    xp = din("xp", [LP, 1024]); xs = din("xs", [128, 1024]); padb = din("padb", [128, 4])
    st0 = din("st0", [NSEQ * 4 * 128, 128]); ptab = din("ptab", [1, NSEQ * NPG], I32)
    ck = din("ck", [NPHYS * 128, 512]); cv = din("cv", [NPHYS * 128, 512]); cik = din("cik", [NPHYS * 128, 64])
    cst = din("cst", [128, 1024]); w1 = din("w1", [1024, A_END]); w2 = din("w2", [1024, B_END])
    w_a = din("w_a", [512, 1024]); w_b = din("w_b", [512, 1024]); w_o = din("w_o", [1024, 1024])
    w_up = din("w_up", [1024, 4096]); w_dn = din("w_dn", [4096, 1024])
    lbl = din("lbl", [2, 512]); hgn = din("hgn", [128, 1]); n1 = din("n1", [1, 1024]); n2 = din("n2", [1, 1024])
    nf = din("nf", [1, 1024])
    yp = dout("yp", [NOWN * 128, 1024]); kp = dout("kp", [NOWN * 128, 512]); vp = dout("vp", [NOWN * 128, 512])
    ikp = dout("ikp", [NOWN * 128, 64]); stp = dout("stp", [4 * 128, 128])
    ys = dout("ys", [128, 1024]); ks = dout("ks", [128, 512]); vs = dout("vs", [128, 512])
    iks = dout("iks", [128, 64]); sts = dout("sts", [NSEQ * 4 * 128, 128])
    kt_scr = dscr("kt_scr", [4, 128, LP], BF16)
    v_scr = dscr("v_scr", [LP, 512], BF16)
    ma_scr = dscr("ma_scr", [(NOWN + 1) * 128, 1024], F32)
    x1_scr = dscr("x1_scr", [(NOWN + 1) * 128, 1024], F32)

    es = ExitStack()
    P = Prog(nc, es)
    op, dma = P.op, P.dma

    W = P.sb("arena", [128, ARENA], BF16)
    U1 = P.sb("u1", [128, 8192], F32)
    JK = P.sb("junk8", [128, 8192], mybir.dt.uint8)
    CF = P.sb("cf", [128, 1024], F32); CB = P.sb("cb", [128, 1024], BF16)
    GG = es.enter_context(nc.sbuf_tensor("gg", [128, 2048], F32))
    G1 = Buf("g1", GG[:, 0:1024]); LB = Buf("lb", GG[:, 1024:1536]); OML = Buf("oml", GG[:, 1536:2048])
    G2 = Buf("g2", GG[:, 0:1024]); GF = Buf("gf", GG[:, 1024:2048])
    HG = P.sb("hg", [128, 1], F32)
    PADB = P.sb("padbs", [128, 4], F32); SS = P.sb("ss", [128, 8], F32)
    XT = [P.sb("xt%d" % i, [128, 1024], F32) for i in range(2)]
    H = P.sb("h", [128, 1024], BF16); HT = P.sb("hT", [128, 8, 128], BF16)
    NSCR = 16 * 1024
    SCR = es.enter_context(nc.sbuf_tensor("scr", [128, NSCR], F32))
    cur = dict(off=0, bufs=[])

    def carve(name, shape, dt):
        n = int(np.prod(shape[1:]))
        nf32 = (n + 1) // 2 if dt == BF16 else n
        nf32 = (nf32 + 7) // 8 * 8
        o = cur['off']
        assert o + nf32 <= NSCR, (name, o, nf32)
        cur['off'] = o + nf32
        ap = SCR[0:shape[0], o:o + nf32]
        if dt == BF16:
            ap = ap.bitcast(BF16)[:, 0:n]
        else:
            ap = ap[:, 0:n]
        if dt == I32:
            ap = SCR[0:shape[0], o:o + nf32].bitcast(I32)[:, 0:n]
        if len(shape) == 3:
            ap = ap.rearrange("p (a b) -> p a b", a=shape[1])
        b = Buf(name, ap)
        cur['bufs'].append(b)
        return b

    def new_pass():
        old = cur['bufs']
        cur['off'] = 0
        cur['bufs'] = []
        return old

    def fence(bufs):
        if bufs:
            op('pool', lambda e: e.memset(SS.t[:, 7:8], 0.0), r=[], w=list(bufs) + [SS])

    PB = [P.ps("pb%d" % i, [128, 512], F32) for i in range(6)]
    PTR = [P.ps("ptr%d" % i, [128, 8, 128], BF16) for i in range(2)]
    st = dict(bank=0, banks=list(range(6)), tr=0, xi=0)

    def bank():
        b = PB[st['banks'][st['bank'] % len(st['banks'])]]
        st['bank'] += 1
        return b

    def trbank():
        b = PTR[st['tr'] % 2]
        st['tr'] += 1
        return b

    IDB = CB.t[:, K_ID:K_ID + 128]

    dma('sp', CF.t[:, :], cst, r=[], w=[CF])
    op('dve', lambda e: e.tensor_copy(out=CB.t[:, :], in_=CF.t[:, :]), r=[CF], w=[CB])
    dma('sp', G1.t[:, :], n1.partition_broadcast(128), r=[], w=[G1])
    dma('sp', LB.t[:, :], lbl[0:1, :].partition_broadcast(128), r=[], w=[LB])
    dma('sp', OML.t[:, :], lbl[1:2, :].partition_broadcast(128), r=[], w=[OML])
    dma('sp', HG.t[:, :], hgn, r=[], w=[HG])
    dma('sp', PADB.t[:, :], padb, r=[], w=[PADB])
    op('dve', lambda e: e.tensor_tensor(out=LB.t[:, :], in0=LB.t[:, :], in1=OML.t[:, :], op=ALU.subtract), r=[LB, OML], w=[LB])
    op('act', lambda e: e.activation(out=LB.t[:, :], in_=LB.t[:, :], func=AF.Sigmoid), r=[LB], w=[LB])
    op('dve', lambda e: e.tensor_scalar(out=OML.t[:, :], in0=LB.t[:, :], scalar1=-1.0, scalar2=1.0, op0=ALU.mult, op1=ALU.add), r=[LB], w=[OML])

    EPSC = P.const_col(EPS)
    WCOLS = dict(n=A_END)

    def Wc(k, off, n):
        return W.t[:, k * WCOLS['n'] + off:k * WCOLS['n'] + off + n]

    def load_w(src, ncols, extra):
        WCOLS['n'] = ncols
        for k in range(8):
            for c0 in range(0, ncols, 2048):
                c1 = min(ncols, c0 + 2048)
                dma('pool', W.t[:, k * ncols + c0:k * ncols + c1], src[k * 128:(k + 1) * 128, c0:c1], r=[], w=[W])
        o = 8 * ncols
        outs = []
        for (ap, nch) in extra:
            for k in range(nch):
                dma('pool', W.t[:, o + k * 1024:o + (k + 1) * 1024], ap[k * 128:(k + 1) * 128, :], r=[], w=[W])
            outs.append(o)
            o += nch * 1024
        assert o <= ARENA
        return outs

    def proj_tok(off, n):
        b = bank()
        for k in range(8):
            op('pe', lambda e, k=k, wap=Wc(k, off, n): e.matmul(b.t[:, 0:n], lhsT=HT.t[:, k, :], rhs=wap, start=(k == 0), stop=(k == 7)),
               r=[HT, W], w=[b])
        return b

    def proj_feat(off, n, b, c0):
        for k in range(8):
            op('pe', lambda e, k=k, wap=Wc(k, off, n): e.matmul(b.t[0:n, c0:c0 + 128], lhsT=wap, rhs=HT.t[:, k, :], start=(k == 0), stop=(k == 7)),
               r=[HT, W], w=[b])

    def rmsnorm_rstd(X, D=1024.0):
        op('dve', lambda e: e.scalar_tensor_tensor(out=JK.t[:, 0:1024], in0=X.t[:, :], scalar=1.0, in1=X.t[:, :], op0=ALU.mult, op1=ALU.mult,
                                                   accum_out=SS.t[:, 0:1]), r=[X], w=[JK, SS])
        op('dve', lambda e: e.tensor_copy(out=SS.t[:, 1:2], in_=SS.t[:, 0:1]), r=[SS], w=[SS])
        op('act', lambda e: e.activation(out=SS.t[:, 2:3], in_=SS.t[:, 1:2], func=AF.Sqrt, scale=1.0 / D,
                                         bias=EPSC), r=[SS], w=[SS])
        op('dve', lambda e: e.reciprocal(out=SS.t[:, 3:4], in_=SS.t[:, 2:3]), r=[SS], w=[SS])
        return SS.t[:, 3:4]

    def to_hT(X, G):
        rstd = rmsnorm_rstd(X)
        op('dve', lambda e: e.scalar_tensor_tensor(out=H.t[:, :], in0=X.t[:, :], scalar=rstd, in1=G.t[:, :], op0=ALU.mult, op1=ALU.mult),
           r=[X, SS, G], w=[H])
        tb = trbank()
        for k in range(8):
            op('pe', lambda e, k=k: e.transpose(out=tb.t[:, k, :], in_=H.t[:, k * 128:(k + 1) * 128], identity=IDB), r=[H, CB], w=[tb])
        op('act', lambda e: e.copy(out=HT.t[:, :, :], in_=tb.t[:, :, :]), r=[tb], w=[HT])

    def load_x(src_ap):
        X = XT[st['xi'] % 2]
        st['xi'] += 1
        dma('sp', X.t[:, :], src_ap, r=[], w=[X])
        return X

    D_V, D_KT, D_IKT, D_MA, D_X1, D_S = (Buf(n, None) for n in ("D_V", "D_KT", "D_IKT", "D_MA", "D_X1", "D_S"))
    ikt_scr = dscr("ikt_scr", [64, LP], BF16)
    kts_scr = dscr("kts_scr", [4, 128, 128], BF16)
    vs_scr = dscr("vs_scr", [128, 512], BF16)
    iks_scr = dscr("iks_scr", [64, 128], BF16)

    woff = load_w(w1, A_END, [(w_a, 4)])
    WA0 = woff[0]
    TP = [carve("tp%d" % i, [128, 512], F32) for i in range(6)]
    tpi = dict(i=0)

    def tmp():
        b = TP[tpi['i'] % len(TP)]
        tpi['i'] += 1
        return b

    LOGF = carve("logf", [128, 512], F32); KK = carve("kk", [128, 512], F32)
    VB = carve("vb", [128, 512], BF16); VA = carve("va", [128, 512], BF16)
    KHAT = carve("khat", [128, 512], BF16); QTL = carve("qtl", [128, 512], BF16); KTL = carve("ktl", [128, 512], BF16)
    QTT = carve("qtT", [128, 4, 128], BF16); KTT = carve("ktT", [128, 4, 128], BF16)
    AT = carve("at", [128, 4, 128], BF16); DEC = carve("dec", [128, 64], F32)
    SF = carve("sf", [128, 4, 128], F32); SBF = [carve("sbf%d" % i, [128, 4, 128], BF16) for i in range(3)]
    SQ = carve("sq", [128, 512], BF16); OF = carve("of", [128, 4, 128], BF16)
    KHC = [carve("khc%d" % i, [128, 512], BF16) for i in range(2)]
    KTILE = carve("ktile", [128, 4, 128], BF16); IKTILE = carve("iktile", [64, 128], BF16)
    IKF = carve("ikf", [128, 64], F32)
    S0F = [carve("s0f%d" % i, [128, 4, 128], F32) for i in range(2)]
    S0B = [carve("s0b%d" % i, [128, 4, 128], BF16) for i in range(2)]
    VM = [carve("vm%d" % i, [128, 512], BF16) for i in range(2)]
    SNEW = [carve("snew%d" % i, [128, 4, 128], F32) for i in range(2)]
    for i in range(3):
        op('pool', lambda e, i=i: e.memset(SBF[i].t[:, :, :], 0.0), r=[], w=[SBF[i]])
    op('pool', lambda e: e.memset(SF.t[:, :, :], 0.0), r=[], w=[SF])
    sidx = dict(i=0)

    CUT = int(_ENV.get("CUT", "0"))

    def pass1_tile(X, kind, j, oi):
        own = kind != 'light'
        samp = kind == 'samp'
        KU, KL, KIND, NCK = (K_U4, K_L4, K_IND4, NSEQ) if samp else (K_U64, K_L64, K_IND64, 2)
        to_hT(X, G1)
        b = proj_tok(A_HF, 512)
        sig = tmp()
        op('act', lambda e: e.activation(out=sig.t[:, :], in_=b.t[:, :], func=AF.Sigmoid), r=[b], w=[sig])
        f = tmp()
        op('pool', lambda e: e.tensor_tensor(out=f.t[:, :], in0=sig.t[:, :], in1=OML.t[:, :], op=ALU.mult), r=[sig, OML], w=[f])
        op('pool', lambda e: e.tensor_tensor(out=f.t[:, :], in0=f.t[:, :], in1=LB.t[:, :], op=ALU.add), r=[f, LB], w=[f])
        op('act', lambda e: e.activation(out=LOGF.t[:, :], in_=f.t[:, :], func=AF.Ln), r=[f], w=[LOGF])
        op('dve', lambda e: e.tensor_scalar(out=KK.t[:, :], in0=f.t[:, :], scalar1=-1.0, scalar2=1.0, op0=ALU.mult, op1=ALU.add), r=[f], w=[KK])
        if CUT == 1:
            return
        b2 = proj_tok(A_HI, 512)
        op('act', lambda e: e.copy(out=VB.t[:, :], in_=b2.t[:, :]), r=[b2], w=[VB])
        b3 = proj_tok(A_AV, 512)
        op('act', lambda e: e.copy(out=VA.t[:, :], in_=b3.t[:, :]), r=[b3], w=[VA])
        if samp:
            dma('sp', vs_scr, VA.t[:, :], r=[VA], w=[], dw=D_S)
        else:
            dma('sp', v_scr[j * 128:(j + 1) * 128, :], VA.t[:, :], r=[VA], w=[], dw=D_V)
        if own:
            t = tmp()
            op('dve', lambda e, t=t: e.tensor_copy(out=t.t[:, :], in_=b3.t[:, :]), r=[b3], w=[t])
            dma('sp', vs if samp else vp[oi * 128:(oi + 1) * 128, :], t.t[:, :], r=[t], w=[])
        if CUT == 2:
            return
        kb = bank()
        for h in range(4):
            proj_feat(A_AK + h * 128, 128, kb, h * 128)
        op('act', lambda e: e.copy(out=KTILE.t[:, :, :], in_=kb.t[:, :].rearrange("p (h t) -> p h t", h=4)), r=[kb], w=[KTILE])
        if samp:
            dma('sp', kts_scr.rearrange("h d t -> d h t"), KTILE.t[:, :, :], r=[KTILE], w=[], dw=D_S)
        else:
            dma('sp', kt_scr[:, :, j * 128:(j + 1) * 128].rearrange("h d t -> d h t"), KTILE.t[:, :, :], r=[KTILE], w=[], dw=D_KT)
        ib = bank()
        proj_feat(A_IK, 64, ib, 0)
        op('act', lambda e: e.copy(out=IKTILE.t[:, :], in_=ib.t[0:64, 0:128]), r=[ib], w=[IKTILE])
        dma('sp', iks_scr if samp else ikt_scr[:, j * 128:(j + 1) * 128], IKTILE.t[:, :], r=[IKTILE], w=[], dw=(D_S if samp else D_IKT))
        if own:
            b4 = proj_tok(A_AK, 512)
            t = tmp()
            op('dve', lambda e, t=t: e.tensor_copy(out=t.t[:, :], in_=b4.t[:, :]), r=[b4], w=[t])
            dma('sp', ks if samp else kp[oi * 128:(oi + 1) * 128, :], t.t[:, :], r=[t], w=[])
            b5 = proj_tok(A_IK, 64)
            op('dve', lambda e: e.tensor_copy(out=IKF.t[:, :], in_=b5.t[:, 0:64]), r=[b5], w=[IKF])
            dma('sp', iks if samp else ikp[oi * 128:(oi + 1) * 128, :], IKF.t[:, :], r=[IKF], w=[])
        if CUT == 3:
            return
        bd = bank()
        op('pe', lambda e: e.matmul(bd.t[:, :], lhsT=CF.t[:, KL:KL + 128], rhs=LOGF.t[:, :], start=True, stop=True), r=[CF, LOGF], w=[bd])
        ebd = tmp()
        op('act', lambda e: e.activation(out=ebd.t[:, :], in_=bd.t[:, :], func=AF.Exp), r=[bd], w=[ebd])
        op('pool', lambda e: e.tensor_tensor(out=KHAT.t[:, :], in0=KK.t[:, :], in1=ebd.t[:, :], op=ALU.mult), r=[KK, ebd], w=[KHAT])
        dc = bank()
        for h in range(4):
            op('pe', lambda e, h=h: e.matmul(dc.t[:, h * NCK:(h + 1) * NCK], lhsT=LOGF.t[:, h * 128:(h + 1) * 128],
                                             rhs=CF.t[:, KIND:KIND + NCK], start=True, stop=True), r=[CF, LOGF], w=[dc])
        op('act', lambda e: e.activation(out=DEC.t[:, 0:4 * NCK], in_=dc.t[:, 0:4 * NCK], func=AF.Exp), r=[dc], w=[DEC])
        if CUT == 11:
            return
        if own:
            bt = bank()
            op('pe', lambda e: e.matmul(bt.t[:, :], lhsT=CF.t[:, KU:KU + 128], rhs=LOGF.t[:, :], start=True, stop=True), r=[CF, LOGF], w=[bt])
            eb = tmp(); enb = tmp()
            op('act', lambda e: e.activation(out=eb.t[:, :], in_=bt.t[:, :], func=AF.Exp), r=[bt], w=[eb])
            op('act', lambda e: e.activation(out=enb.t[:, :], in_=bt.t[:, :], func=AF.Exp, scale=-1.0), r=[bt], w=[enb])
            if CUT == 12:
                return
            hq = proj_tok(A_HQ, 512)
            op('dve', lambda e: e.tensor_tensor(out=QTL.t[:, :], in0=hq.t[:, :], in1=eb.t[:, :], op=ALU.mult), r=[hq, eb], w=[QTL])
            op('pool', lambda e: e.tensor_tensor(out=KTL.t[:, :], in0=KK.t[:, :], in1=enb.t[:, :], op=ALU.mult), r=[KK, enb], w=[KTL])
            if CUT == 13:
                return
            tb = trbank()
            for h in range(4):
                op('pe', lambda e, h=h: e.transpose(out=tb.t[:, h, :], in_=QTL.t[:, h * 128:(h + 1) * 128], identity=IDB), r=[QTL, CB], w=[tb])
            for h in range(4):
                op('pe', lambda e, h=h: e.transpose(out=tb.t[:, 4 + h, :], in_=KTL.t[:, h * 128:(h + 1) * 128], identity=IDB), r=[KTL, CB], w=[tb])
            if CUT == 14:
                return
            op('act', lambda e: e.copy(out=QTT.t[:, :, :], in_=tb.t[:, 0:4, :]), r=[tb], w=[QTT])
            if CUT == 15:
                return
            op('act', lambda e: e.copy(out=KTT.t[:, :, :], in_=tb.t[:, 4:8, :]), r=[tb], w=[KTT])
            if CUT == 7:
                return
            ab = bank()
            for h in range(4):
                op('pe', lambda e, h=h: e.matmul(ab.t[:, h * 128:(h + 1) * 128], lhsT=KTT.t[:, h, :], rhs=QTT.t[:, h, :], start=True, stop=True),
                   r=[KTT, QTT], w=[ab])
            for h in range(4):
                op('dve', lambda e, h=h: e.tensor_tensor(out=AT.t[:, h, :], in0=ab.t[:, h * 128:(h + 1) * 128], in1=CF.t[:, KU:KU + 128], op=ALU.mult),
                   r=[ab, CF], w=[AT])
        if CUT == 4:
            return
        obh = None
        if not samp:
            si = sidx['i']
            s_start, s_mid, s_end = SBF[si % 3], SBF[(si + 1) % 3], SBF[(si + 2) % 3]
            sidx['i'] += 2
            for c, s_out in ((0, s_mid), (1, s_end)):
                op('pool', lambda e, c=c: e.tensor_scalar(out=KHC[c].t[:, :], in0=KHAT.t[:, :], scalar1=CF.t[:, K_IND64 + c:K_IND64 + c + 1], scalar2=None,
                                                          op0=ALU.mult), r=[KHAT, CF], w=[KHC[c]])
                ub = bank()
                for h in range(4):
                    op('pe', lambda e, h=h, c=c, ub=ub: e.matmul(ub.t[:, h * 128:(h + 1) * 128], lhsT=KHC[c].t[:, h * 128:(h + 1) * 128],
                                                         rhs=VB.t[:, h * 128:(h + 1) * 128], start=True, stop=True),
                       r=[KHC[c], VB], w=[ub])
                for h in range(4):
                    op('dve', lambda e, h=h, c=c, ub=ub: e.scalar_tensor_tensor(out=SF.t[:, h, :], in0=SF.t[:, h, :], scalar=DEC.t[:, h * 2 + c:h * 2 + c + 1],
                                                                      in1=ub.t[:, h * 128:(h + 1) * 128], op0=ALU.mult, op1=ALU.add),
                       r=[SF, DEC, ub], w=[SF])
                op('act', lambda e, s_out=s_out: e.copy(out=s_out.t[:, :, :], in_=SF.t[:, :, :]), r=[SF], w=[s_out])
            if CUT == 8:
                return
            if own:
                ob = bank()
                for h in range(4):
                    op('pe', lambda e, h=h: e.matmul(ob.t[:, h * 128:(h + 1) * 128], lhsT=VB.t[:, h * 128:(h + 1) * 128], rhs=AT.t[:, h, :],
                                                     start=True, stop=False), r=[VB, AT], w=[ob])
                    op('pe', lambda e, h=h: e.matmul(ob.t[:, h * 128:h * 128 + 64], lhsT=s_start.t[:, h, :], rhs=QTT.t[:, h, 0:64],
                                                     start=False, stop=False), r=[s_start, QTT], w=[ob])
                    op('pe', lambda e, h=h: e.matmul(ob.t[:, h * 128 + 64:h * 128 + 128], lhsT=s_mid.t[:, h, :], rhs=QTT.t[:, h, 64:128],
                                                     start=False, stop=True), r=[s_mid, QTT], w=[ob])
                obh = [(ob, h * 128) for h in range(4)]
        else:
            obs = [bank() for _ in range(4)]
            st['banks'] = [i for i in range(6) if all(PB[i] is not o_ for o_ in obs)]
            for h in range(4):
                op('pe', lambda e, h=h: e.matmul(obs[h].t[:, 0:128], lhsT=VB.t[:, h * 128:(h + 1) * 128], rhs=AT.t[:, h, :],
                                                 start=True, stop=False), r=[VB, AT], w=[obs[h]])
            for bq in range(NSEQ):
                sf = S0F[bq % 2]; sb_ = S0B[bq % 2]; vm = VM[bq % 2]; sn = SNEW[bq % 2]
                dma('sp', sf.t[:, :, :], st0[bq * 512:(bq + 1) * 512, :].rearrange("(h p) e -> p h e", p=128), r=[], w=[sf])
                op('act', lambda e, sf=sf, sb_=sb_: e.copy(out=sb_.t[:, :, :], in_=sf.t[:, :, :]), r=[sf], w=[sb_])
                for h in range(4):
                    op('pe', lambda e, h=h, bq=bq, sb_=sb_: e.matmul(obs[h].t[:, 4 * bq:4 * bq + 4], lhsT=sb_.t[:, h, :], rhs=QTT.t[:, h, 4 * bq:4 * bq + 4],
                                                                 start=False, stop=(bq == NSEQ - 1)), r=[sb_, QTT], w=[obs[h]])
                op('pool', lambda e, bq=bq, vm=vm: e.tensor_scalar(out=vm.t[:, :], in0=VB.t[:, :], scalar1=CF.t[:, K_IND4 + bq:K_IND4 + bq + 1], scalar2=None,
                                                               op0=ALU.mult), r=[VB, CF], w=[vm])
                ub = bank()
                for h in range(4):
                    op('pe', lambda e, h=h, vm=vm, ub=ub: e.matmul(ub.t[:, h * 128:(h + 1) * 128], lhsT=KHAT.t[:, h * 128:(h + 1) * 128],
                                                           rhs=vm.t[:, h * 128:(h + 1) * 128], start=True, stop=True), r=[KHAT, vm], w=[ub])
                for h in range(4):
                    op('dve', lambda e, h=h, bq=bq, sf=sf, sn=sn, ub=ub: e.scalar_tensor_tensor(
                        out=sn.t[:, h, :], in0=sf.t[:, h, :], scalar=DEC.t[:, h * NCK + bq:h * NCK + bq + 1],
                        in1=ub.t[:, h * 128:(h + 1) * 128], op0=ALU.mult, op1=ALU.add), r=[sf, DEC, ub], w=[sn])
                dma('sp', sts[bq * 512:(bq + 1) * 512, :].rearrange("(h p) e -> p h e", p=128), sn.t[:, :, :], r=[sn], w=[])
            obh = [(obs[h], 0) for h in range(4)]
        if not own:
            return
        if CUT == 9:
            return
        for h in range(4):
            ob_, c0 = obh[h]
            op('act', lambda e, h=h, ob_=ob_, c0=c0: e.activation(out=SQ.t[:, h * 128:(h + 1) * 128], in_=ob_.t[:, c0:c0 + 128], func=AF.Square),
               r=[ob_], w=[SQ])
        ssb = bank()
        op('pe', lambda e: e.matmul(ssb.t[:, :], lhsT=CB.t[:, K_ONES:K_ONES + 128], rhs=SQ.t[:, :], start=True, stop=True), r=[CB, SQ], w=[ssb])
        ro = tmp()
        op('act', lambda e: e.activation(out=ro.t[:, :], in_=ssb.t[:, :], func=AF.Sqrt, scale=1.0 / 128, bias=EPSC), r=[ssb], w=[ro])
        op('dve', lambda e: e.reciprocal(out=ro.t[:, :], in_=ro.t[:, :]), r=[ro], w=[ro])
        gb_ = bank()
        for h in range(4):
            proj_feat(A_HOG + h * 128, 128, gb_, h * 128)
        sil = tmp()
        op('act', lambda e: e.activation(out=sil.t[:, :], in_=gb_.t[:, :], func=AF.Silu), r=[gb_], w=[sil])
        o1 = tmp()
        for h in range(4):
            ob_, c0 = obh[h]
            op('dve', lambda e, h=h, ob_=ob_, c0=c0: e.tensor_tensor(out=o1.t[:, h * 128:(h + 1) * 128], in0=ob_.t[:, c0:c0 + 128],
                                                                 in1=ro.t[:, h * 128:(h + 1) * 128], op=ALU.mult), r=[ob_, ro], w=[o1])
        st['banks'] = list(range(6))
        op('dve', lambda e: e.scalar_tensor_tensor(out=OF.t[:, :, :], in0=o1.t[:, :].rearrange("p (h t) -> p h t", h=4), scalar=HG.t[:, 0:1],
                                                   in1=sil.t[:, :].rearrange("p (h t) -> p h t", h=4), op0=ALU.mult, op1=ALU.mult),
           r=[o1, HG, sil], w=[OF])
        if CUT == 10:
            return
        for half in range(2):
            yb = bank()
            for h in range(4):
                op('pe', lambda e, h=h, half=half, yb=yb: e.matmul(yb.t[:, :], lhsT=OF.t[:, h, :],
                                                           rhs=W.t[:, WA0 + h * 1024 + half * 512:WA0 + h * 1024 + half * 512 + 512],
                                                           start=(h == 0), stop=(h == 3)), r=[OF, W], w=[yb])
            gab = proj_tok(A_GA + half * 512, 512)
            sg = tmp()
            op('act', lambda e, gab=gab, sg=sg: e.activation(out=sg.t[:, :], in_=gab.t[:, :], func=AF.Sigmoid), r=[gab], w=[sg])
            ma = tmp()
            op('dve', lambda e, yb=yb, sg=sg, ma=ma: e.tensor_tensor(out=ma.t[:, :], in0=yb.t[:, :], in1=sg.t[:, :], op=ALU.mult), r=[yb, sg], w=[ma])
            dma('sp', ma_scr[oi * 128:(oi + 1) * 128, half * 512:(half + 1) * 512], ma.t[:, :], r=[ma], w=[], dw=D_MA)

    STOP = int(_ENV.get("STOP", "99"))
    for j in range(min(NT, STOP)):
        X = load_x(xp[j * 128:(j + 1) * 128, :])
        if j % 4 == 3:
            pass1_tile(X, 'own', j, j // 4)
        else:
            pass1_tile(X, 'light', j, None)
    dma('sp', stp.rearrange("(h p) e -> p h e", p=128), SF.t[:, :, :], r=[SF], w=[])
    if STOP > 10:
        X = load_x(xs)
        pass1_tile(X, 'samp', None, NOWN)

    PASSES = int(_ENV.get("PASSES", "3"))
    if PASSES >= 2:
        old = new_pass()
        TP[:] = [carve("q%d" % i, [128, 512], F32) for i in range(5)]
        MK = [carve("mk%d" % i, [128, 512], BF16) for i in range(2)]
        KTC = [carve("ktc%d" % i, [128, 4, 512], BF16) for i in range(2)]
        VC = [carve("vc%d" % i, [128, 4, 512], BF16) for i in range(2)]
        IKC = [carve("ikc%d" % i, [64, 512], BF16) for i in range(2)]
        EX = [carve("ex%d" % i, [128, 512], BF16) for i in range(2)]
        PM = [carve("pm%d" % i, [128, 512], BF16) for i in range(2)]
        PTT = [carve("ptt%d" % i, [128, 4, 128], BF16) for i in range(2)]
        AQT = carve("aqT", [128, 4, 128], BF16); IQT = carve("iqT", [64, 8, 128], BF16)
        IW = carve("iw", [128, 8], F32); IWS = carve("iws", [128, 8], F32); THR = carve("thr", [128, 16], F32)
        NCHR = max(LP // 512, NPG + 1)
        RS = carve("rs", [128, 4 * NCHR], F32)
        AO = carve("ao", [128, 512], BF16); AOT = carve("aoT", [128, 4, 128], BF16)
        MB = carve("mb", [128, 1024], BF16); MT = carve("mT", [128, 8, 128], BF16)
        NPT = NSEQ * NPG
        PTI = carve("pti", [128, NPT], I32); PTF = carve("ptf", [128, NPT], F32); PIDX = carve("pidx", [128, NPT], I32)
        IOTA = carve("iota", [128, 8], I32); IOTF = carve("iotf", [128, 8], F32)
        PGB = [carve("pgb%d" % i, [128, 512], BF16) for i in range(2)]
        IPB = [carve("ipb%d" % i, [128, 64], BF16) for i in range(2)]
        GB = [carve("gb%d" % i, [128, 512], F32) for i in range(4)]
        GI = [carve("gi%d" % i, [128, 64], F32) for i in range(2)]
        fence(old + cur['bufs'] + [W])
        woff = load_w(w2, B_END, [(w_b, 4), (w_o, 8)])
        WB0, WO0 = woff
        IDX_SCALE = 512.0 ** -0.5
        rr = dict(i=0)

        def ring(lst):
            k = id(lst)
            rr[k] = rr.get(k, 0) + 1
            return lst[rr[k] % len(lst)]

        dma('sp', PTI.t[:, :], ptab.partition_broadcast(128), r=[], w=[PTI])
        op('pool', lambda e: e.iota(IOTA.t[:, 0:1], pattern=[[0, 1]], base=0, channel_multiplier=1), r=[], w=[IOTA])
        op('dve', lambda e: e.tensor_copy(out=PTF.t[:, :], in_=PTI.t[:, :]), r=[PTI], w=[PTF])
        op('dve', lambda e: e.tensor_copy(out=IOTF.t[:, 0:1], in_=IOTA.t[:, 0:1]), r=[IOTA], w=[IOTF])
        op('dve', lambda e: e.tensor_scalar(out=PTF.t[:, :], in0=PTF.t[:, :], scalar1=128.0, scalar2=IOTF.t[:, 0:1], op0=ALU.mult, op1=ALU.add),
           r=[PTF, IOTF], w=[PTF])
        op('dve', lambda e: e.tensor_copy(out=PIDX.t[:, :], in_=PTF.t[:, :]), r=[PTF], w=[PIDX])

        class PromptProv:
            def ik(self, c, n):
                b = ring(IKC)
                dma('sp', b.t[0:64, 0:n], ikt_scr[:, c * 512:c * 512 + n], r=[], w=[b], dr=D_IKT)
                return b

            def kv(self, c, n):
                ktc = ring(KTC); vc = ring(VC)
                dma('sp', ktc.t[:, :, 0:n], kt_scr[:, :, c * 512:c * 512 + n].rearrange("h d l -> d h l"), r=[], w=[ktc], dr=D_KT)
                dma('sp', vc.t[:, 0:n // 128, :], v_scr[c * 512:c * 512 + n, :].rearrange("(t p) e -> p t e", p=128), r=[], w=[vc], dr=D_V)
                return ktc, vc

        class SampProv:
            def __init__(self, bq):
                self.bq = bq

            def ik(self, c, n):
                bq = self.bq
                b = ring(IKC)
                nt = n // 128
                npg = sum(1 for tl in range(nt) if 4 * c + tl < NPG)
                tb = trbank() if npg else None
                for tl in range(nt):
                    tile = 4 * c + tl
                    if tile < NPG:
                        pg = ring(GI); ipb = ring(IPB)
                        dma('pool', pg.t[:, 0:64], cik, r=[PIDX], w=[pg], indirect=PIDX.t[:, bq * NPG + tile:bq * NPG + tile + 1])
                        op('act', lambda e, pg=pg, ipb=ipb: e.copy(out=ipb.t[:, :], in_=pg.t[:, 0:64]), r=[pg], w=[ipb])
                        op('pe', lambda e, tb=tb, ipb=ipb, tl=tl: e.transpose(out=tb.t[0:64, tl, :], in_=ipb.t[:, 0:64], identity=IDB), r=[ipb, CB], w=[tb])
                    else:
                        op('pool', lambda e, b=b, tl=tl: e.memset(b.t[0:64, tl * 128:(tl + 1) * 128], 0.0), r=[], w=[b])
                        dma('sp', b.t[0:64, tl * 128:tl * 128 + 4], iks_scr[:, 4 * bq:4 * bq + 4], r=[], w=[b], dr=D_S)
                if npg:
                    op('act', lambda e, tb=tb, b=b, npg=npg: e.copy(out=b.t[0:64, 0:npg * 128], in_=tb.t[0:64, 0:npg, :]), r=[tb], w=[b])
                return b

            def kv(self, c, n):
                bq = self.bq
                ktc = ring(KTC); vc = ring(VC)
                nt = n // 128
                for tl in range(nt):
                    tile = 4 * c + tl
                    if tile < NPG:
                        col = PIDX.t[:, bq * NPG + tile:bq * NPG + tile + 1]
                        pg = ring(GB)
                        dma('pool', pg.t[:, :], ck, r=[PIDX], w=[pg], indirect=col)
                        pgb = ring(PGB)
                        op('act', lambda e, pg=pg, pgb=pgb: e.copy(out=pgb.t[:, :], in_=pg.t[:, :]), r=[pg], w=[pgb])
                        tb = trbank()
                        for h in range(4):
                            op('pe', lambda e, h=h, tb=tb, pgb=pgb: e.transpose(out=tb.t[:, h, :], in_=pgb.t[:, h * 128:(h + 1) * 128], identity=IDB),
                               r=[pgb, CB], w=[tb])
                        op('act', lambda e, tb=tb, ktc=ktc, tl=tl: e.copy(out=ktc.t[:, :, tl * 128:(tl + 1) * 128], in_=tb.t[:, 0:4, :]), r=[tb], w=[ktc])
                        pg2 = ring(GB)
                        dma('pool', pg2.t[:, :], cv, r=[PIDX], w=[pg2], indirect=col)
                        op('dve', lambda e, pg2=pg2, vc=vc, tl=tl: e.tensor_copy(out=vc.t[:, tl, :], in_=pg2.t[:, :]), r=[pg2], w=[vc])
                    else:
                        op('pool', lambda e, ktc=ktc, tl=tl: e.memset(ktc.t[:, :, tl * 128:(tl + 1) * 128], 0.0), r=[], w=[ktc])
                        dma('sp', ktc.t[:, :, tl * 128:tl * 128 + 4], kts_scr[:, :, 4 * bq:4 * bq + 4].rearrange("h d t -> d h t"), r=[], w=[ktc], dr=D_S)
                        op('pool', lambda e, vc=vc, tl=tl: e.memset(vc.t[:, tl, :], 0.0), r=[], w=[vc])
                        dma('sp', vc.t[0:4, tl, :], vs_scr[4 * bq:4 * bq + 4, :], r=[], w=[vc], dr=D_S)
                return ktc, vc

        SCb = U1

        def attend(nq, qc0, iwb, L, ksel, prov, CH, prompt):
            SC = U1.t
            nch = (L + CH - 1) // CH
            IDQ = CB.t[0:nq, K_ID:K_ID + nq]
            for c in range(nch):
                n = min(CH, L - c * CH)
                ikc = prov.ik(c, n)
                for h in range(8):
                    b = bank()
                    op('pe', lambda e, b=b, h=h, ikc=ikc, n=n: e.matmul(b.t[0:nq, 0:n], lhsT=IQT.t[0:64, h, qc0:qc0 + nq], rhs=ikc.t[0:64, 0:n],
                                                                       start=True, stop=True), r=[IQT, ikc], w=[b])
                    rl = tmp()
                    op('act', lambda e, b=b, rl=rl, n=n: e.activation(out=rl.t[0:nq, 0:n], in_=b.t[0:nq, 0:n], func=AF.Relu), r=[b], w=[rl])
                    if h == 0:
                        op('dve', lambda e, rl=rl, c=c, n=n: e.tensor_scalar(out=SC[0:nq, c * CH:c * CH + n], in0=rl.t[0:nq, 0:n], scalar1=iwb.t[0:nq, 0:1],
                                                                          scalar2=None, op0=ALU.mult), r=[rl, iwb], w=[SCb])
                    else:
                        op('dve', lambda e, rl=rl, c=c, n=n, h=h: e.scalar_tensor_tensor(out=SC[0:nq, c * CH:c * CH + n], in0=rl.t[0:nq, 0:n],
                                                                                      scalar=iwb.t[0:nq, h:h + 1], in1=SC[0:nq, c * CH:c * CH + n],
                                                                                      op0=ALU.mult, op1=ALU.add), r=[rl, iwb, SCb], w=[SCb])
            T = THR.t
            op('dve', lambda e: e.tensor_reduce(out=T[0:nq, 0:1], in_=SC[0:nq, 0:L], axis=AX.X, op=ALU.max), r=[SCb], w=[THR])
            op('dve', lambda e: e.tensor_reduce(out=T[0:nq, 1:2], in_=SC[0:nq, 0:L], axis=AX.X, op=ALU.min), r=[SCb], w=[THR])
            if prompt:
                for t in range(3):
                    op('dve', lambda e, t=t: e.tensor_scalar(out=SC[0:nq, t * 128:(t + 1) * 128], in0=SC[0:nq, t * 128:(t + 1) * 128],
                                                            scalar1=PADB.t[0:nq, t:t + 1], scalar2=None, op0=ALU.add), r=[SCb, PADB], w=[SCb])
            op('dve', lambda e: e.tensor_tensor(out=SC[0:nq, L - 128:L], in0=SC[0:nq, L - 128:L], in1=CF.t[0:nq, K_CNEG:K_CNEG + 128], op=ALU.add),
               r=[SCb, CF], w=[SCb])
            op('dve', lambda e: e.tensor_tensor(out=T[0:nq, 2:3], in0=T[0:nq, 0:1], in1=T[0:nq, 1:2], op=ALU.subtract), r=[THR], w=[THR])
            op('dve', lambda e: e.tensor_copy(out=T[0:nq, 3:4], in_=T[0:nq, 1:2]), r=[THR], w=[THR])
            for it in range(1, NIT + 1):
                sc = 2.0 ** -it
                op('dve', lambda e, sc=sc: e.scalar_tensor_tensor(out=T[0:nq, 4:5], in0=T[0:nq, 2:3], scalar=sc, in1=T[0:nq, 3:4], op0=ALU.mult, op1=ALU.add),
                   r=[THR], w=[THR])
                op('dve', lambda e: e.tensor_scalar(out=JK.t[0:nq, 0:L], in0=SC[0:nq, 0:L], scalar1=T[0:nq, 4:5], scalar2=None, op0=ALU.is_ge, op1=ALU.add,
                                                    accum_out=T[0:nq, 5:6]), r=[SCb, THR], w=[JK, THR])
                op('dve', lambda e: e.tensor_scalar(out=T[0:nq, 6:7], in0=T[0:nq, 5:6], scalar1=float(ksel) - 0.5, scalar2=None, op0=ALU.is_ge),
                   r=[THR], w=[THR])
                op('dve', lambda e: e.tensor_tensor(out=T[0:nq, 7:8], in0=T[0:nq, 6:7], in1=T[0:nq, 2:3], op=ALU.mult), r=[THR], w=[THR])
                op('dve', lambda e, sc=sc: e.scalar_tensor_tensor(out=T[0:nq, 3:4], in0=T[0:nq, 7:8], scalar=sc, in1=T[0:nq, 3:4], op0=ALU.mult, op1=ALU.add),
                   r=[THR], w=[THR])
            st['banks'] = [4, 5]
            OA = PB[0:4]
            op('pool', lambda e: e.memset(RS.t[:, :], 0.0), r=[], w=[RS])
            for c in range(nch):
                n = min(CH, L - c * CH)
                nt = n // 128
                ktc, vc = prov.kv(c, n)
                mk = ring(MK)
                op('dve', lambda e, mk=mk, c=c, n=n: e.tensor_scalar(out=mk.t[0:nq, 0:n], in0=SC[0:nq, c * CH:c * CH + n], scalar1=T[0:nq, 3:4], scalar2=None,
                                                                  op0=ALU.is_ge), r=[SCb, THR], w=[mk])
                for h in range(4):
                    lg = bank()
                    op('pe', lambda e, lg=lg, h=h, ktc=ktc, n=n: e.matmul(lg.t[0:nq, 0:n], lhsT=AQT.t[:, h, qc0:qc0 + nq], rhs=ktc.t[:, h, 0:n],
                                                                         start=True, stop=True), r=[AQT, ktc], w=[lg])
                    ex = ring(EX); pm = ring(PM); pt = ring(PTT)
                    op('act', lambda e, lg=lg, ex=ex, n=n: e.activation(out=ex.t[0:nq, 0:n], in_=lg.t[0:nq, 0:n], func=AF.Exp), r=[lg], w=[ex])
                    op('dve', lambda e, ex=ex, pm=pm, mk=mk, n=n, c=c, h=h: e.scalar_tensor_tensor(
                        out=pm.t[0:nq, 0:n], in0=ex.t[0:nq, 0:n], scalar=1.0, in1=mk.t[0:nq, 0:n], op0=ALU.mult, op1=ALU.mult,
                        accum_out=RS.t[0:nq, h * NCHR + c:h * NCHR + c + 1]), r=[ex, mk], w=[pm, RS])
                    tb = trbank()
                    for t in range(nt):
                        op('pe', lambda e, tb=tb, pm=pm, t=t: e.transpose(out=tb.t[:, t, 0:nq], in_=pm.t[0:nq, t * 128:(t + 1) * 128], identity=IDQ),
                           r=[pm, CB], w=[tb])
                    op('act', lambda e, tb=tb, pt=pt, nt=nt: e.copy(out=pt.t[:, 0:nt, 0:nq], in_=tb.t[:, 0:nt, 0:nq]), r=[tb], w=[pt])
                    for t in range(nt):
                        op('pe', lambda e, pt=pt, vc=vc, t=t, h=h, c=c, nt=nt: e.matmul(
                            OA[h].t[0:nq, 0:128], lhsT=pt.t[:, t, 0:nq], rhs=vc.t[:, t, h * 128:(h + 1) * 128],
                            start=(c == 0 and t == 0), stop=(c == nch - 1 and t == nt - 1)), r=[pt, vc], w=[OA[h]])
            if _ENV.get("DBG") == "1" and not prompt and qc0 == 0:
                dma('sp', ks[0:4, 0:L], U1.t[0:4, 0:L], r=[U1], w=[])
                dma('sp', ks[4:8, 0:16], THR.t[0:4, 0:16], r=[THR], w=[])
                dma('sp', ks[8:12, 0:4 * nch], RS.t[0:4, 0:4 * nch], r=[RS], w=[])
            op('dve', lambda e: e.tensor_reduce(out=T[0:nq, 8:12], in_=RS.t[0:nq, 0:4 * NCHR].rearrange("p (h c) -> p h c", h=4), axis=AX.X, op=ALU.add),
               r=[RS], w=[THR])
            op('dve', lambda e: e.reciprocal(out=T[0:nq, 12:16], in_=T[0:nq, 8:12]), r=[THR], w=[THR])
            for h in range(4):
                op('act', lambda e, h=h: e.activation(out=AO.t[0:nq, h * 128:(h + 1) * 128], in_=OA[h].t[0:nq, 0:128], func=AF.Copy,
                                                      scale=T[0:nq, 12 + h:13 + h]), r=[OA[h], THR], w=[AO])
            tb = trbank()
            for h in range(4):
                op('pe', lambda e, tb=tb, h=h: e.transpose(out=tb.t[:, h, 0:nq], in_=AO.t[0:nq, h * 128:(h + 1) * 128], identity=IDQ), r=[AO, CB], w=[tb])
            op('act', lambda e, tb=tb: e.copy(out=AOT.t[:, :, qc0:qc0 + nq], in_=tb.t[:, 0:4, 0:nq]), r=[tb], w=[AOT])
            st['banks'] = list(range(6))

        def pass2_tile(X, oi, samp):
            to_hT(X, G1)
            ab = bank()
            for h in range(4):
                proj_feat(B_AQ + h * 128, 128, ab, h * 128)
            op('act', lambda e: e.activation(out=AQT.t[:, :, :], in_=ab.t[:, :].rearrange("p (h t) -> p h t", h=4), func=AF.Copy, scale=128.0 ** -0.5),
               r=[ab], w=[AQT])
            for g in range(2):
                ibk = bank()
                for hh in range(4):
                    proj_feat(B_IQ + (g * 4 + hh) * 64, 64, ibk, hh * 128)
                op('act', lambda e, g=g, ibk=ibk: e.copy(out=IQT.t[0:64, g * 4:(g + 1) * 4, :], in_=ibk.t[0:64, :].rearrange("p (h t) -> p h t", h=4)),
                   r=[ibk], w=[IQT])
            wb_ = proj_tok(B_IW, 8)
            op('dve', lambda e: e.tensor_scalar(out=IW.t[:, :], in0=wb_.t[:, 0:8], scalar1=IDX_SCALE, scalar2=None, op0=ALU.mult), r=[wb_], w=[IW])
            if not samp:
                attend(128, 0, IW, (4 * oi + 4) * 128, KSEL_P, PromptProv(), 512, True)
            else:
                op('pool', lambda e: e.memset(AOT.t[:, :, :], 0.0), r=[], w=[AOT])
                for bq in range(NSEQ):
                    dma('sp', IWS.t[0:4, :], IW.t[4 * bq:4 * bq + 4, :], r=[IW], w=[IWS])
                    attend(4, 4 * bq, IWS, LS, KSEL_S, SampProv(bq), 512, False)
            if _ENV.get("DBG") == "2" and samp:
                dma('pool', ks[:, :].rearrange("p (h t) -> p h t", h=4), AOT.t[:, :, :], r=[AOT], w=[])
            for half in range(2):
                yb = bank()
                for h in range(4):
                    op('pe', lambda e, h=h, half=half, yb=yb: e.matmul(yb.t[:, :], lhsT=AOT.t[:, h, :],
                                                                      rhs=W.t[:, WB0 + h * 1024 + half * 512:WB0 + h * 1024 + half * 512 + 512],
                                                                      start=(h == 0), stop=(h == 3)), r=[AOT, W], w=[yb])
                gbb = proj_tok(B_GB + half * 512, 512)
                sg = tmp(); m = tmp(); mat = tmp()
                op('act', lambda e, gbb=gbb, sg=sg: e.activation(out=sg.t[:, :], in_=gbb.t[:, :], func=AF.Sigmoid), r=[gbb], w=[sg])
                op('dve', lambda e, yb=yb, sg=sg, m=m: e.tensor_tensor(out=m.t[:, :], in0=yb.t[:, :], in1=sg.t[:, :], op=ALU.mult), r=[yb, sg], w=[m])
                dma('sp', mat.t[:, :], ma_scr[oi * 128:(oi + 1) * 128, half * 512:(half + 1) * 512], r=[], w=[mat], dr=D_MA)
                op('pool', lambda e, m=m, mat=mat, half=half: e.tensor_tensor(out=MB.t[:, half * 512:(half + 1) * 512], in0=m.t[:, :], in1=mat.t[:, :], op=ALU.add),
                   r=[m, mat], w=[MB])
            if _ENV.get("DBG") == "3" and samp:
                dma('pool', ks[:, :], MB.t[:, 0:512], r=[MB], w=[])
                dma('pool', vs[:, :], MB.t[:, 512:1024], r=[MB], w=[])
            tb = trbank()
            for k in range(8):
                op('pe', lambda e, k=k, tb=tb: e.transpose(out=tb.t[:, k, :], in_=MB.t[:, k * 128:(k + 1) * 128], identity=IDB), r=[MB, CB], w=[tb])
            op('act', lambda e, tb=tb: e.copy(out=MT.t[:, :, :], in_=tb.t[:, :, :]), r=[tb], w=[MT])
            for half in range(2):
                xb = bank()
                for k in range(8):
                    op('pe', lambda e, k=k, half=half, xb=xb: e.matmul(xb.t[:, :], lhsT=MT.t[:, k, :],
                                                                      rhs=W.t[:, WO0 + k * 1024 + half * 512:WO0 + k * 1024 + half * 512 + 512],
                                                                      start=(k == 0), stop=(k == 7)), r=[MT, W], w=[xb])
                x1 = tmp()
                op('dve', lambda e, xb=xb, x1=x1, half=half, X=X: e.tensor_tensor(out=x1.t[:, :], in0=xb.t[:, :], in1=X.t[:, half * 512:(half + 1) * 512], op=ALU.add),
                   r=[xb, X], w=[x1])
                dma('sp', x1_scr[oi * 128:(oi + 1) * 128, half * 512:(half + 1) * 512], x1.t[:, :], r=[x1], w=[], dw=D_X1)
                if PASSES == 2:
                    dst = ys if samp else yp[oi * 128:(oi + 1) * 128, :]
                    dma('sp', dst[:, half * 512:(half + 1) * 512], x1.t[:, :], r=[x1], w=[])

        for oi in range(NOWN):
            X = load_x(xp[(4 * oi + 3) * 128:(4 * oi + 4) * 128, :])
            pass2_tile(X, oi, False)
        X = load_x(xs)
        pass2_tile(X, NOWN, True)

    if PASSES >= 3:
        old = new_pass()
        TP[:] = [carve("r%d" % i, [128, 512], F32) for i in range(4)]
        X1T = [carve("x1t%d" % i, [128, 1024], F32) for i in range(4)]
        H2T = carve("h2T", [128, 8, 512], BF16)
        X2 = [carve("x2_%d" % i, [128, 1024], F32) for i in range(2)]
        YT = [carve("yt%d" % i, [128, 1024], F32) for i in range(2)]
        HID = U1.t[:, 0:8192].bitcast(BF16).rearrange("p (c t) -> p c t", c=32)
        fence(old + cur['bufs'] + [W, U1, G1, LB, OML, G2, GF])
        dma('sp', G2.t[:, :], n2.partition_broadcast(128), r=[], w=[G2])
        dma('sp', GF.t[:, :], nf.partition_broadcast(128), r=[], w=[GF])
        NTL = NOWN + 1
        GS = int(_ENV.get("GS", "4"))
        for g0 in range(0, NTL, GS):
            tiles = list(range(g0, min(NTL, g0 + GS)))
            ntok = len(tiles) * 128
            for ti, t in enumerate(tiles):
                Xt = X1T[ti]
                dma('sp', Xt.t[:, :], x1_scr[t * 128:(t + 1) * 128, :], r=[], w=[Xt], dr=D_X1)
                rstd = rmsnorm_rstd(Xt)
                op('dve', lambda e, Xt=Xt, rstd=rstd: e.scalar_tensor_tensor(out=H.t[:, :], in0=Xt.t[:, :], scalar=rstd, in1=G2.t[:, :], op0=ALU.mult, op1=ALU.mult),
                   r=[Xt, SS, G2], w=[H])
                tb = trbank()
                for k in range(8):
                    op('pe', lambda e, k=k, tb=tb: e.transpose(out=tb.t[:, k, :], in_=H.t[:, k * 128:(k + 1) * 128], identity=IDB), r=[H, CB], w=[tb])
                op('act', lambda e, tb=tb, ti=ti: e.copy(out=H2T.t[:, :, ti * 128:(ti + 1) * 128], in_=tb.t[:, :, :]), r=[tb], w=[H2T])
            for k in range(8):
                for c0 in range(0, 4096, 2048):
                    dma('pool', W.t[:, k * 4096 + c0:k * 4096 + c0 + 2048], w_up[k * 128:(k + 1) * 128, c0:c0 + 2048], r=[], w=[W])
            for c in range(32):
                hb = bank()
                for k in range(8):
                    op('pe', lambda e, k=k, c=c, hb=hb, ntok=ntok: e.matmul(hb.t[:, 0:ntok], lhsT=W.t[:, k * 4096 + c * 128:k * 4096 + (c + 1) * 128], rhs=H2T.t[:, k, 0:ntok],
                                                              start=(k == 0), stop=(k == 7)), r=[W, H2T], w=[hb])
                rl = tmp()
                op('act', lambda e, hb=hb, rl=rl, ntok=ntok: e.activation(out=rl.t[:, 0:ntok], in_=hb.t[:, 0:ntok], func=AF.Relu), r=[hb], w=[rl])
                op('pool', lambda e, rl=rl, c=c, ntok=ntok: e.tensor_tensor(out=HID[:, c, 0:ntok], in0=rl.t[:, 0:ntok], in1=rl.t[:, 0:ntok], op=ALU.mult), r=[rl], w=[U1])
                if _ENV.get("DBG") == "6" and c == 0 and g0 == 0:
                    dma('sp', ks[:, 0:ntok], rl.t[:, 0:ntok], r=[rl], w=[])
                    dma('pool', vs[:, 0:ntok], HID[:, 0, 0:ntok], r=[U1], w=[])
                    dma('pool', iks[:, 0:64], H2T.t[:, 0, 192:256], r=[H2T], w=[])
            for c in range(32):
                dma('pool', W.t[:, c * 1024:(c + 1) * 1024], w_dn[c * 128:(c + 1) * 128, :], r=[], w=[W])
            for ti, t in enumerate(tiles):
                Xt = X1T[ti]
                x2 = X2[ti % 2]; yt = YT[ti % 2]
                for half in range(2):
                    yb = bank()
                    for c in range(32):
                        op('pe', lambda e, c=c, half=half, yb=yb, ti=ti: e.matmul(yb.t[:, :], lhsT=HID[:, c, ti * 128:(ti + 1) * 128],
                                                                             rhs=W.t[:, c * 1024 + half * 512:c * 1024 + half * 512 + 512],
                                                                             start=(c == 0), stop=(c == 31)), r=[U1, W], w=[yb])
                    op('dve', lambda e, yb=yb, x2=x2, Xt=Xt, half=half: e.tensor_tensor(out=x2.t[:, half * 512:(half + 1) * 512], in0=yb.t[:, :],
                                                                                   in1=Xt.t[:, half * 512:(half + 1) * 512], op=ALU.add), r=[yb, Xt], w=[x2])
                rstd = rmsnorm_rstd(x2)
                op('dve', lambda e, x2=x2, yt=yt, rstd=rstd: e.scalar_tensor_tensor(out=yt.t[:, :], in0=x2.t[:, :], scalar=rstd, in1=GF.t[:, :], op0=ALU.mult, op1=ALU.mult),
                   r=[x2, SS, GF], w=[yt])
                DBGV = _ENV.get("DBG")
                src = {"4": Xt, "5": x2}.get(DBGV, yt)
                dma('sp', ys if t == NOWN else yp[t * 128:(t + 1) * 128, :], src.t[:, :], r=[src], w=[])

    P.finish()
    es.close()
    return nc


FULL_CFG = dict(NT=64, NSEQ=16, NPG=16, NPHYS=2560, KSEL_P=256, KSEL_S=256)
_NC_CACHE = {}


def _prep_core(c, cfg, inp):
    NT, NSEQ, NPG, NPHYS = cfg['NT'], cfg['NSEQ'], cfg['NPG'], cfg['NPHYS']
    s, r = c // 4, c % 4
    pad = 3 - r
    f32 = np.float32
    xp = np.zeros((NT * 128, 1024), f32)
    xp[pad * 128:] = inp['x_prompt'][s, :(NT - pad) * 128]
    padb = np.zeros((128, 4), f32)
    padb[:, :pad] = -BIG
    xs = np.zeros((128, 1024), f32)
    xs[:4 * NSEQ] = inp['x_sample'][c * NSEQ:(c + 1) * NSEQ].reshape(4 * NSEQ, 1024)
    wi = inp['w_in'][0]
    col = lambda name, n: wi[:, R_OFF[name]:R_OFF[name] + n]
    w1 = np.concatenate([col('hf', 512), col('hi', 512), col('av', 512), col('ak', 512), col('ik', 64),
                         col('hq', 512), col('hog', 512), col('ga', 1024)], axis=1)
    w2 = np.concatenate([col('aq', 512), col('iq', 512), col('iw', 8), col('gb', 1024)], axis=1)
    d = dict(
        xp=xp, xs=xs, padb=padb,
        st0=inp['state_hgrn'][0, c * NSEQ:(c + 1) * NSEQ].reshape(NSEQ * 512, 128),
        ptab=inp['page_table'][c * NSEQ:(c + 1) * NSEQ].reshape(1, NSEQ * NPG).astype(np.int32),
        ck=inp['cache_k'][0].reshape(NPHYS * 128, 512), cv=inp['cache_v'][0].reshape(NPHYS * 128, 512),
        cik=inp['cache_idx_k'][0].reshape(NPHYS * 128, 64),
        cst=make_consts(), w1=w1, w2=w2, w_a=inp['w_a'][0], w_b=inp['w_b'][0], w_o=inp['w_o'][0],
        w_up=inp['w_up'][0], w_dn=inp['w_down'][0], lbl=inp['lb_logits'], hgn=inp['hg_norm'][0].reshape(128, 1),
        n1=inp['norm1'][0].reshape(1, 1024), n2=inp['norm2'][0].reshape(1, 1024), nf=inp['norm_f'].reshape(1, 1024))
    return {k: np.ascontiguousarray(v) for k, v in d.items()}


def run_cfg(cfg, inp, names=None):
    key = tuple(sorted(cfg.items()))
    if key not in _NC_CACHE:
        _NC_CACHE[key] = build(cfg)
    nc = _NC_CACHE[key]
    cores = cfg.get('cores', tuple(range(8)))
    in_maps = [_prep_core(c, cfg, inp) for c in cores]
    res = run_bass_kernel_spmd(nc, in_maps, core_ids=list(range(len(cores))))
    out = [None] * 8
    for i, c in enumerate(cores):
        out[c] = res.results[i]
    return out


def assemble(cfg, res):
    NT, NSEQ = cfg['NT'], cfg['NSEQ']
    NOWN = NT // 4
    SEQ = NT * 128
    f32 = np.float32
    y_p = np.zeros((2, SEQ, 1024), f32); k_p = np.zeros((1, 2, SEQ, 4, 128), f32); v_p = np.zeros((1, 2, SEQ, 4, 128), f32)
    i_p = np.zeros((1, 2, SEQ, 64), f32); s_p = np.zeros((1, 2, 4, 128, 128), f32)
    nb = 8 * NSEQ
    y_s = np.zeros((nb, 4, 1024), f32); k_s = np.zeros((1, nb, 4, 4, 128), f32); v_s = np.zeros((1, nb, 4, 4, 128), f32)
    i_s = np.zeros((1, nb, 4, 64), f32); s_s = np.zeros((1, nb, 4, 128, 128), f32)
    for c in range(8):
        s, r = c // 4, c % 4
        o = res[c]
        if o is None:
            continue
        for oi in range(NOWN):
            j = 4 * oi + r
            sl = slice(j * 128, (j + 1) * 128)
            so = slice(oi * 128, (oi + 1) * 128)
            if 'yp' in o:
                y_p[s, sl] = o['yp'][so]
            k_p[0, s, sl] = o['kp'][so].reshape(128, 4, 128)
            v_p[0, s, sl] = o['vp'][so].reshape(128, 4, 128)
            i_p[0, s, sl] = o['ikp'][so]
        if r == 3:
            s_p[0, s] = o['stp'].reshape(4, 128, 128)
        bs = slice(c * NSEQ, (c + 1) * NSEQ)
        if 'ys' in o:
            y_s[bs] = o['ys'][:4 * NSEQ].reshape(NSEQ, 4, 1024)
        k_s[0, bs] = o['ks'][:4 * NSEQ].reshape(NSEQ, 4, 4, 128)
        v_s[0, bs] = o['vs'][:4 * NSEQ].reshape(NSEQ, 4, 4, 128)
        i_s[0, bs] = o['iks'][:4 * NSEQ].reshape(NSEQ, 4, 64)
        s_s[0, bs] = o['sts'].reshape(NSEQ, 4, 128, 128)
    return (y_p, y_s, k_p, v_p, i_p, s_p, k_s, v_s, i_s, s_s)


def kernel(**inputs):
    inp = {k: np.asarray(v) for k, v in inputs.items()}
    res = run_cfg(FULL_CFG, inp)
    return assemble(FULL_CFG, res)
```
